# Optimizing a Trainium2 kernel written in Bass

```python
import math
import jax, jax.numpy as jnp
from jax import lax
import numpy as np

D_MODEL = 2048
BATCH = 4
SEQ = 8192
DEPTH = 1
DEC_BATCH = 2
DEC_SEQ = 16384
PAST_LEN = 128

HEAD_DIM = 64
N_Q_HEADS = 16
N_KV_HEADS = 4
Q_PER_KV = N_Q_HEADS // N_KV_HEADS
ATTN_WIDTH = N_Q_HEADS * HEAD_DIM
KV_WIDTH = N_KV_HEADS * HEAD_DIM
WINDOW = 128
BLOCK_Q = 128
ROT_DIM = HEAD_DIM // 4
ROPE_THETA = 500000.0
HY_WIDTH = D_MODEL - ATTN_WIDTH
HY_ORDER = 2
SHORT_CONV = 3
HY_EMB_DIM = 33
HY_FILTER_HIDDEN = 64
HY_DECAY_TARGET = 1e-2
HY_FAST_DECAY_PCT = 0.3
HY_SLOW_DECAY_PCT = 1.5
IN_WIDTH = ATTN_WIDTH + 2 * KV_WIDTH + (HY_ORDER + 1) * HY_WIDTH
N_EXPERT_GROUPS = 4
EXPERTS_PER_GROUP = 8
N_EXPERTS = N_EXPERT_GROUPS * EXPERTS_PER_GROUP
TOP_K = 2
D_EXPERT = 1024
MOE_BLOCK = 128
EPS = 1e-6

kernel_name = 'hybrid_hyena_swa_hmoe_encoder'


def rms_norm(x, w):
    xf = x.astype(jnp.float32)
    y = xf * lax.rsqrt(jnp.mean(xf * xf, axis=-1, keepdims=True) + EPS)
    return (y * w.astype(jnp.float32)).astype(x.dtype)


def partial_rope(x):
    L = x.shape[1]
    half = ROT_DIM // 2
    inv_freq = jnp.power(ROPE_THETA, -jnp.arange(half, dtype=jnp.float32) * 2.0 / ROT_DIM)
    ang = jnp.arange(L, dtype=jnp.float32)[:, None] * inv_freq[None, :]
    cos = jnp.cos(ang)[None, :, None, :]
    sin = jnp.sin(ang)[None, :, None, :]
    xf = x.astype(jnp.float32)
    x1 = xf[..., :half]
    x2 = xf[..., half:ROT_DIM]
    out = jnp.concatenate([x1 * cos - x2 * sin, x2 * cos + x1 * sin, xf[..., ROT_DIM:]], axis=-1)
    return out.astype(x.dtype)


def windowed_gqa(q, k, v, sink):
    B, L = q.shape[0], q.shape[1]
    nb = L // BLOCK_Q
    span = BLOCK_Q + 2 * WINDOW
    qb = q.reshape(B, nb, BLOCK_Q, N_KV_HEADS, Q_PER_KV, HEAD_DIM).transpose(1, 0, 2, 3, 4, 5)
    pad = ((0, 0), (WINDOW, WINDOW), (0, 0), (0, 0))
    kp = jnp.pad(k, pad)
    vp = jnp.pad(v, pad)
    sink_g = sink.astype(jnp.float32).reshape(N_KV_HEADS, Q_PER_KV)[None, :, :, None]
    scale = HEAD_DIM ** -0.5

    def one_block(args):
        i, qi = args
        start = i * BLOCK_Q
        ki = lax.dynamic_slice_in_dim(kp, start, span, axis=1)
        vi = lax.dynamic_slice_in_dim(vp, start, span, axis=1)
        s = jnp.einsum('bqkgd,bskd->bkgqs', qi, ki).astype(jnp.float32) * scale
        qpos = start + jnp.arange(BLOCK_Q)
        kpos = start - WINDOW + jnp.arange(span)
        ok = (jnp.abs(kpos[None, :] - qpos[:, None]) <= WINDOW) & ((kpos >= 0) & (kpos < L))[None, :]
        s = jnp.where(ok, s, -jnp.inf)
        m = jnp.maximum(jnp.max(s, axis=-1), sink_g)
        p = jnp.exp(s - m[..., None])
        denom = jnp.sum(p, axis=-1) + jnp.exp(sink_g - m)
        p = (p / denom[..., None]).astype(vi.dtype)
        return jnp.einsum('bkgqs,bskd->bqkgd', p, vi)

    out = lax.map(one_block, (jnp.arange(nb), qb))
    return out.transpose(1, 0, 2, 3, 4, 5).reshape(B, L, ATTN_WIDTH)


def hyena_filter_fft(L, w1, b1, w2, b2, w3, b3, w4, freq):
    f32 = jnp.float32
    t = jnp.linspace(0.0, 1.0, L, dtype=f32)[:, None]
    bands = (HY_EMB_DIM - 1) // 2
    fr = jnp.linspace(1e-4, bands - 1, bands, dtype=f32)
    w = 2.0 * math.pi * jnp.arange(L, dtype=f32) / L
    ang = w[:, None] * fr[None, :]
    z = jnp.concatenate([t, jnp.cos(ang), -jnp.sin(ang)], axis=-1)
    a = freq.astype(f32)
    h = jnp.sin(a * (z @ w1.astype(f32) + b1.astype(f32)))
    h = jnp.sin(a * (h @ w2.astype(f32) + b2.astype(f32)))
    h = jnp.sin(a * (h @ w3.astype(f32) + b3.astype(f32)))
    h = (h @ w4.astype(f32)).reshape(L, 2, HY_ORDER, HY_WIDTH)
    max_decay = math.log(HY_DECAY_TARGET) / HY_FAST_DECAY_PCT
    min_decay = math.log(HY_DECAY_TARGET) / HY_SLOW_DECAY_PCT
    deltas = jnp.linspace(min_decay, max_decay, HY_WIDTH, dtype=f32)
    decay = jnp.exp(-t * jnp.abs(deltas)[None, :])
    h = h * decay[:, None, None, :]
    h_fwd = h[:, 0]
    h_bwd = h[:, 1]
    g = jnp.concatenate([h_fwd, jnp.zeros((1, HY_ORDER, HY_WIDTH), f32), h_bwd[1:][::-1]], axis=0)
    g = g / jnp.sum(jnp.abs(g), axis=0, keepdims=True)
    return jnp.fft.rfft(g, axis=0)


def hyena_mixer(u, conv_w, conv_b, filt_fft, hy_bias):
    L = u.shape[1]
    up = jnp.pad(u, ((0, 0), (1, 1), (0, 0)))
    uc = conv_w[0] * up[:, :-2] + conv_w[1] * up[:, 1:-1] + conv_w[2] * up[:, 2:] + conv_b
    gates = (uc[..., :HY_WIDTH], uc[..., HY_WIDTH:2 * HY_WIDTH])
    z = uc[..., 2 * HY_WIDTH:].astype(jnp.float32)
    for n in range(HY_ORDER):
        zf = jnp.fft.rfft(z, n=2 * L, axis=1)
        conv = jnp.fft.irfft(zf * filt_fft[None, :, n, :], n=2 * L, axis=1)[:, :L]
        z = gates[n].astype(jnp.float32) * (conv + z * hy_bias[n].astype(jnp.float32))
    return z.astype(u.dtype)


def hier_moe(x2d, w_rg, b_rg, w_re, b_re, w_gate, w_up, w_down):
    T = x2d.shape[0]
    f32 = jnp.float32
    xf = x2d.astype(f32)
    g_logits = xf @ w_rg.astype(f32) + b_rg.astype(f32)
    g_prob = jax.nn.softmax(g_logits, axis=-1)
    g_idx = jnp.argmax(g_logits, axis=-1)
    g_gate = jnp.take_along_axis(g_prob, g_idx[:, None], axis=1)
    e_logits = (xf @ w_re.astype(f32) + b_re.astype(f32)).reshape(T, N_EXPERT_GROUPS, EXPERTS_PER_GROUP)
    e_logits = jnp.take_along_axis(e_logits, g_idx[:, None, None], axis=1)[:, 0]
    top_val, top_loc = lax.top_k(e_logits, TOP_K)
    weights = g_gate * jax.nn.softmax(top_val, axis=-1)
    experts = g_idx[:, None] * EXPERTS_PER_GROUP + top_loc

    A = T * TOP_K
    flat_e = experts.reshape(-1)
    flat_w = weights.reshape(-1)
    order = jnp.argsort(flat_e)
    sorted_e = flat_e[order]
    tok = (order // TOP_K).astype(jnp.int32)
    counts = jnp.bincount(flat_e, length=N_EXPERTS)
    seg_start = jnp.cumsum(counts) - counts
    padded = ((counts + MOE_BLOCK - 1) // MOE_BLOCK) * MOE_BLOCK
    pend = jnp.cumsum(padded)
    pstart = pend - padded
    dest = pstart[sorted_e] + (jnp.arange(A) - seg_start[sorted_e])
    n_blocks = (A + MOE_BLOCK - 1) // MOE_BLOCK + N_EXPERTS
    P = n_blocks * MOE_BLOCK
    buf_tok = jnp.full((P,), T, jnp.int32).at[dest].set(tok)
    buf_w = jnp.zeros((P,), f32).at[dest].set(flat_w[order])
    block_expert = jnp.minimum(jnp.searchsorted(pend, jnp.arange(n_blocks) * MOE_BLOCK, side='right'), N_EXPERTS - 1)
    x_pad = jnp.concatenate([x2d, jnp.zeros((1, x2d.shape[1]), x2d.dtype)], axis=0)

    def expert_block(b):
        rows = lax.dynamic_slice_in_dim(buf_tok, b * MOE_BLOCK, MOE_BLOCK)
        e = block_expert[b]
        xb = x_pad[rows]
        hid = jax.nn.silu(xb @ w_gate[e]) * (xb @ w_up[e])
        return hid @ w_down[e]

    yb = lax.map(expert_block, jnp.arange(n_blocks)).reshape(P, -1)
    out = jnp.zeros((T + 1, x2d.shape[1]), f32).at[buf_tok].add(yb.astype(f32) * buf_w[:, None])
    return out[:T].astype(x2d.dtype)


def encoder_layer(x, norm1_w, w_in, q_norm_w, k_norm_w, attn_sink, conv_w, conv_b,
                  filt_w1, filt_b1, filt_w2, filt_b2, filt_w3, filt_b3, filt_w4, filt_freq, hy_bias,
                  attn_out_norm_w, hy_out_norm_w, w_out, norm2_w,
                  w_route_group, b_route_group, w_route_expert, b_route_expert, w_gate, w_up, w_down):
    B, L, D = x.shape
    h = rms_norm(x, norm1_w)
    proj = h @ w_in
    o1 = ATTN_WIDTH
    o2 = o1 + KV_WIDTH
    o3 = o2 + KV_WIDTH
    q = proj[..., :o1].reshape(B, L, N_Q_HEADS, HEAD_DIM)
    k = proj[..., o1:o2].reshape(B, L, N_KV_HEADS, HEAD_DIM)
    v = proj[..., o2:o3].reshape(B, L, N_KV_HEADS, HEAD_DIM)
    u = proj[..., o3:]
    q = partial_rope(rms_norm(q, q_norm_w))
    k = partial_rope(rms_norm(k, k_norm_w))
    y_attn = windowed_gqa(q, k, v, attn_sink)
    filt_fft = hyena_filter_fft(L, filt_w1, filt_b1, filt_w2, filt_b2, filt_w3, filt_b3, filt_w4, filt_freq)
    y_hy = hyena_mixer(u, conv_w, conv_b, filt_fft, hy_bias)
    mixed = jnp.concatenate([rms_norm(y_attn, attn_out_norm_w), rms_norm(y_hy, hy_out_norm_w)], axis=-1)
    x = x + mixed @ w_out
    h2 = rms_norm(x, norm2_w).reshape(B * L, D)
    x = x + hier_moe(h2, w_route_group, b_route_group, w_route_expert, b_route_expert,
                     w_gate, w_up, w_down).reshape(B, L, D)
    return x


def setup_inputs(seed: int = 0) -> dict:
    key = jax.random.key(seed)
    ks = jax.random.split(key, 32)

    def nrm(k, shape, s):
        return jax.random.normal(k, shape, jnp.float32) * s

    FW = 2 * HY_ORDER * HY_WIDTH
    return {
        'x_prompt': nrm(ks[0], (BATCH, SEQ, D_MODEL), 1.0),
        'x_sample': nrm(ks[1], (DEC_BATCH, DEC_SEQ, D_MODEL), 1.0),
        'norm1_w': 1.0 + nrm(ks[2], (DEPTH, D_MODEL), 0.01),
        'w_in': nrm(ks[3], (DEPTH, D_MODEL, IN_WIDTH), D_MODEL ** -0.5),
        'q_norm_w': 1.0 + nrm(ks[4], (DEPTH, HEAD_DIM), 0.01),
        'k_norm_w': 1.0 + nrm(ks[5], (DEPTH, HEAD_DIM), 0.01),
        'attn_sink': nrm(ks[6], (DEPTH, N_Q_HEADS), 0.5),
        'conv_w': nrm(ks[7], (DEPTH, SHORT_CONV, 3 * HY_WIDTH), SHORT_CONV ** -0.5),
        'conv_b': nrm(ks[8], (DEPTH, 3 * HY_WIDTH), 0.02),
        'filt_w1': nrm(ks[9], (DEPTH, HY_EMB_DIM, HY_FILTER_HIDDEN), HY_EMB_DIM ** -0.5),
        'filt_b1': nrm(ks[10], (DEPTH, HY_FILTER_HIDDEN), 0.02),
        'filt_w2': nrm(ks[11], (DEPTH, HY_FILTER_HIDDEN, HY_FILTER_HIDDEN), HY_FILTER_HIDDEN ** -0.5),
        'filt_b2': nrm(ks[12], (DEPTH, HY_FILTER_HIDDEN), 0.02),
        'filt_w3': nrm(ks[13], (DEPTH, HY_FILTER_HIDDEN, HY_FILTER_HIDDEN), HY_FILTER_HIDDEN ** -0.5),
        'filt_b3': nrm(ks[14], (DEPTH, HY_FILTER_HIDDEN), 0.02),
        'filt_w4': nrm(ks[15], (DEPTH, HY_FILTER_HIDDEN, FW), HY_FILTER_HIDDEN ** -0.5),
        'filt_freq': 1.0 + nrm(ks[16], (DEPTH, HY_FILTER_HIDDEN), 0.01),
        'hy_bias': nrm(ks[17], (DEPTH, HY_ORDER, HY_WIDTH), 1.0),
        'attn_out_norm_w': 1.0 + nrm(ks[18], (DEPTH, ATTN_WIDTH), 0.01),
        'hy_out_norm_w': 1.0 + nrm(ks[19], (DEPTH, HY_WIDTH), 0.01),
        'w_out': nrm(ks[20], (DEPTH, D_MODEL, D_MODEL), D_MODEL ** -0.5),
        'norm2_w': 1.0 + nrm(ks[21], (DEPTH, D_MODEL), 0.01),
        'w_route_group': nrm(ks[22], (DEPTH, D_MODEL, N_EXPERT_GROUPS), D_MODEL ** -0.5),
        'b_route_group': nrm(ks[23], (DEPTH, N_EXPERT_GROUPS), 0.01),
        'w_route_expert': nrm(ks[24], (DEPTH, D_MODEL, N_EXPERTS), D_MODEL ** -0.5),
        'b_route_expert': nrm(ks[25], (DEPTH, N_EXPERTS), 0.01),
        'w_gate': nrm(ks[26], (DEPTH, N_EXPERTS, D_MODEL, D_EXPERT), D_MODEL ** -0.5),
        'w_up': nrm(ks[27], (DEPTH, N_EXPERTS, D_MODEL, D_EXPERT), D_MODEL ** -0.5),
        'w_down': nrm(ks[28], (DEPTH, N_EXPERTS, D_EXPERT, D_MODEL), D_EXPERT ** -0.5),
    }


def reference(x_prompt, x_sample, norm1_w, w_in, q_norm_w, k_norm_w, attn_sink, conv_w, conv_b,
              filt_w1, filt_b1, filt_w2, filt_b2, filt_w3, filt_b3, filt_w4, filt_freq, hy_bias,
              attn_out_norm_w, hy_out_norm_w, w_out, norm2_w,
              w_route_group, b_route_group, w_route_expert, b_route_expert, w_gate, w_up, w_down):
    def trunk(x):
        for l in range(DEPTH):
            x = encoder_layer(x, norm1_w[l], w_in[l], q_norm_w[l], k_norm_w[l], attn_sink[l],
                              conv_w[l], conv_b[l], filt_w1[l], filt_b1[l], filt_w2[l], filt_b2[l],
                              filt_w3[l], filt_b3[l], filt_w4[l], filt_freq[l], hy_bias[l],
                              attn_out_norm_w[l], hy_out_norm_w[l], w_out[l], norm2_w[l],
                              w_route_group[l], b_route_group[l], w_route_expert[l], b_route_expert[l],
                              w_gate[l], w_up[l], w_down[l])
        return x

    y_prompt = trunk(x_prompt)
    y_sample = trunk(x_sample)
    return (y_prompt, y_sample)
```

```python
import math
from contextlib import ExitStack
import numpy as np
import concourse.bass as bass
import concourse.mybir as mybir
from concourse.bass_utils import run_bass_kernel_spmd

F32 = mybir.dt.float32
BF16 = mybir.dt.bfloat16
I32 = mybir.dt.int32
U32 = mybir.dt.uint32
AF = mybir.ActivationFunctionType
ALU = mybir.AluOpType
AX = mybir.AxisListType

ENGS = ("tensor", "vector", "scalar", "gpsimd", "sync")
SAME_ENGINE_SYNC = True
EPS = 1e-6


class Obj:
    __slots__ = ("name", "t", "w", "r", "sem", "is_dram")

    def __init__(self, name, t=None, is_dram=False):
        self.name = name
        self.t = t
        self.w = {}
        self.r = {}
        self.sem = None
        self.is_dram = is_dram

    def __getitem__(self, idx):
        return self.t[idx]


class Rec:
    def __init__(self, nc, es, n_dma_sems=92):
        self.nc = nc
        self.sems = {}
        self.val = {}
        for e in ENGS:
            self.sems[e] = es.enter_context(nc.semaphore("S_" + e))
            self.val[e] = 0
        self.free_dma = {"hw": [], "sw": []}
        for i in range(n_dma_sems):
            k = "D%d" % i
            self.sems[k] = es.enter_context(nc.semaphore(k))
            self.val[k] = 0
            self.free_dma["hw" if i % 2 == 0 else "sw"].append(k)
        self.phase_dma = []
        self.ops = {e: [] for e in ENGS}
        self.seen = {e: {} for e in ENGS}
        self.nops = 0
        self.sem_sw = {}
        self.uid = 0

    def sb(self, st, name, shape, dt):
        self.uid += 1
        name = "%s_u%d" % (name, self.uid)
        return Obj(name, st.enter_context(self.nc.sbuf_tensor(name, list(shape), dt)))

    def ps(self, st, name, shape, dt):
        self.uid += 1
        name = "%s_u%d" % (name, self.uid)
        return Obj(name, st.enter_context(self.nc.psum_tensor(name, list(shape), dt)))

    def dram(self, name, shape, dt, kind="Internal"):
        t = self.nc.dram_tensor(name, list(shape), dt, kind=kind)
        return Obj(name, t.ap(), is_dram=True)

    def _dsem(self, o, kind):
        if o.sem is None:
            o.sem = {}
        if kind not in o.sem:
            o.sem[kind] = self.free_dma[kind].pop()
            self.phase_dma.append((o, kind))
        return o.sem[kind]

    def _deps(self, eng, reads, writes):
        need = {}
        for o in reads:
            for k, v in o.w.items():
                if need.get(k, 0) < v:
                    need[k] = v
        for o in writes:
            for d in (o.w, o.r):
                for k, v in d.items():
                    if need.get(k, 0) < v:
                        need[k] = v
        waits = []
        seen = self.seen[eng]
        for k, v in need.items():
            if k == eng and (eng == "tensor" or not SAME_ENGINE_SYNC):
                continue
            if seen.get(k, 0) >= v:
                continue
            seen[k] = v
            waits.append((k, v))
        return waits

    def _mark(self, reads, writes, key, value):
        for o in reads:
            if o.r.get(key, 0) < value:
                o.r[key] = value
        for o in writes:
            o.w = {key: value}
            o.r = {}

    def op(self, eng, fn, reads=(), writes=()):
        waits = self._deps(eng, reads, writes)
        self.val[eng] += 1
        self.ops[eng].append((waits, fn, eng, 1))
        self._mark(reads, writes, eng, self.val[eng])
        self.nops += 1

    def dma(self, q, fn, reads=(), writes=(), semobj=None):
        if semobj is None:
            semobj = [o for o in list(writes) + list(reads) if not o.is_dram][0]
        key = self._dsem(semobj, "sw" if q == "gpsimd" else "hw")
        waits = self._deps(q, reads, writes)
        if q == "gpsimd" and self.val[key] > 0 and self.seen[q].get(key, 0) < self.val[key]:
            self.seen[q][key] = self.val[key]
            waits.append((key, self.val[key]))
        self.val[key] += 16
        self.ops[q].append((waits, fn, key, 16))
        self._mark(reads, writes, key, self.val[key])
        self.nops += 1

    def I(self, eng, meth, reads, writes, *a, **kw):
        self.op(eng, lambda e: getattr(e, meth)(*a, **kw), reads, writes)

    def D(self, q, reads, writes, semobj=None, **kw):
        self.dma(q, lambda e: e.dma_start(**kw), reads, writes, semobj)

    def flush(self):
        for e in ENGS:
            waits = []
            for k, v in self.val.items():
                if v > 0 and self.seen[e].get(k, 0) < v and k != e:
                    self.seen[e][k] = v
                    waits.append((k, v))
            self.ops[e].append((waits, None, None, 0))
        sems = self.sems
        ops = self.ops
        with self.nc.Block() as block:
            def mk(ename):
                def body(e):
                    for waits, fn, key, inc in ops[ename]:
                        for k, v in waits:
                            e.wait_ge(sems[k], v)
                        if fn is not None:
                            fn(e).then_inc(sems[key], inc)
                return body
            block.tensor(mk("tensor"))
            block.vector(mk("vector"))
            block.scalar(mk("scalar"))
            block.gpsimd(mk("gpsimd"))
            block.sync(mk("sync"))
        self.ops = {e: [] for e in ENGS}
        for o, kind in self.phase_dma:
            self.free_dma[kind].append(o.sem.pop(kind))
        self.phase_dma = []


class Rot:
    def __init__(self, items):
        self.items = items
        self.i = 0

    def next(self):
        o = self.items[self.i % len(self.items)]
        self.i += 1
        return o


class Cfg:
    def __init__(self, TOK=8192, NE=32, EPG=8, DE=1024, CAP=640, debug=()):
        self.TOK = TOK
        self.NB = TOK // 128
        self.NT = TOK // 512
        self.TOKX = TOK + 512
        self.D = 2048
        self.NE = NE
        self.EPG = EPG
        self.NG = NE // EPG
        self.DE = DE
        self.CAP = CAP
        self.NSLOT = NE * CAP
        self.debug = tuple(debug)


def build(cfg):
    nc = bass.Bass("TRN2", target_bir_lowering=False)
    TOK, NB, NT, TOKX, D = cfg.TOK, cfg.NB, cfg.NT, cfg.TOKX, cfg.D
    es = ExitStack()
    R = Rec(nc, es)

    def ein(name, shape, dt=F32):
        return R.dram(name, shape, dt, kind="ExternalInput")

    x_own = ein("x_own", [TOKX, D])
    x_oth = ein("x_oth", [TOK, D])
    w_in = ein("w_in", [D, 4608])
    n1w = ein("n1w", [128, D])
    ident = ein("ident", [128, 128])
    rotm = ein("rotm", [128, 128])
    blk1 = ein("blk1", [128, 128])
    qkw = ein("qkw", [128, 2])
    cos_t = ein("cos_t", [128, TOKX])
    sin_t = ein("sin_t", [128, TOKX])
    NG, NE, EPG = cfg.NG, cfg.NE, cfg.EPG
    NGE = NG + NE
    masks = ein("masks", [4, 128, 512])
    sink2 = ein("sink2", [128, 8])
    w_out = ein("w_out", [D, D])
    aonw = ein("aonw", [128, 8])
    hynw = ein("hynw", [128, 8])
    n2w = ein("n2w", [128, D])
    w_r = ein("w_r", [D, NGE])
    b_r = ein("b_r", [128, NGE])
    iota_e = ein("iota_e", [128, NE])
    DE, CAP, NSLOT = cfg.DE, cfg.CAP, cfg.NSLOT
    CAPB = CAP // 128
    w_gate = ein("w_gate", [NE, D, DE])
    w_up = ein("w_up", [NE, D, DE])
    w_down = ein("w_down", [NE, DE, D])
    trim = ein("trim", [128, 128])
    tokid = ein("tokid", [128, NB])
    y_out = R.dram("y", [TOK, D], F32, kind="ExternalOutput")
    SLOTI = R.dram("SLOTI", [NSLOT + 128, 2], F32)
    Yd = R.dram("Yd", [NSLOT + 128, D], F32)
    N = 2 * TOK
    NHI = N // 128
    KL = NHI // 2 + 1
    CG = 64
    cwt = ein("cwt", [128, 24, 4])
    eflag = ein("eflag", [128, 4])
    f_w1 = ein("f_w1", [33, 64])
    f_w2 = ein("f_w2", [64, 64])
    f_w3 = ein("f_w3", [64, 64])
    f_w4 = ein("f_w4", [64, 4096])
    f_vec = ein("f_vec", [64, 4])
    f_zt = ein("f_zt", [3, 33, N])
    f_tt = ein("f_tt", [3, N])
    f_cnt = ein("f_cnt", [128, 3 * (N // 512) + 3])
    f_dsel = ein("f_dsel", [64, 12])
    f_delta = ein("f_delta", [128, 8])
    hyb = ein("hyb", [64, 2, 16])
    m_f1 = ein("m_f1", [NHI, 2 * KL])
    m_f2 = ein("m_f2", [128, KL, 2, 128])
    m_i1 = ein("m_i1", [128, 2, 256])
    m_i2 = ein("m_i2", [KL, 128, 2, NB])
    UC = R.dram("UC", [3072, 2, TOK], BF16)
    TAPS = R.dram("TAPS", [3, 2, 1024, N], BF16)
    RN = R.dram("RN", [128, 16], F32)
    GSP = R.dram("GSP", [5, 16, 128, KL * 2 * CG], BF16)
    GSW = R.dram("GSW", [5, 16, 128, KL * 2 * CG], BF16)
    XSP = R.dram("XSP", [2, 16, 128, KL * 2 * CG], BF16)
    Z1 = R.dram("Z1", [1024, 2, TOK], BF16)
    YH = R.dram("YH", [1024, TOK], BF16)
    X1 = R.dram("X1", [TOK, D], F32)
    H2 = R.dram("H2", [TOK + 128, D], BF16)
    Qs = R.dram("Qs", [16, 64, TOKX], BF16)
    Ks = R.dram("Ks", [4, 64, TOKX], BF16)
    Vs = R.dram("Vs", [TOKX, 256], BF16)
    dbg = {}

    def mk_norm_T(st, c_n1w, c_idb, c_eps):
        xb = Rot([R.sb(st, "xb%d" % i, [128, D], F32) for i in range(2)])
        junk = R.sb(st, "junk", [128, D], BF16)
        hb = Rot([R.sb(st, "hb%d" % i, [128, D], BF16) for i in range(2)])
        stat = Rot([R.sb(st, "stat%d" % i, [128, 4], F32) for i in range(4)])
        pT = Rot([R.ps(st, "pT%d" % i, [128, 8, 128], BF16) for i in range(2)])
        evq = [0]

        def load_norm_transpose(xsrc, row0, hTt, blk):
            xt = xb.next()
            R.D("sync", [xsrc], [xt], out=xt[:], in_=xsrc[row0:row0 + 128, :])
            s = stat.next()
            R.I("scalar", "activation", [xt], [junk, s], out=junk[:], in_=xt[:], func=AF.Square, accum_out=s[:, 0:1])
            R.I("scalar", "activation", [s, c_eps], [s], out=s[:, 1:2], in_=s[:, 0:1], func=AF.Sqrt, bias=c_eps[:, 0:1], scale=1.0 / D)
            R.I("vector", "reciprocal", [s], [s], out=s[:, 2:3], in_=s[:, 1:2])
            h = hb.next()
            R.I("vector", "scalar_tensor_tensor", [xt, s, c_n1w], [h], out=h[:], in0=xt[:], scalar=s[:, 2:3], in1=c_n1w[:],
                op0=ALU.mult, op1=ALU.mult)
            for half in range(2):
                p = pT.next()
                for c in range(8):
                    cc = half * 8 + c
                    R.I("tensor", "transpose", [h, c_idb], [p], out=p[:, c, :], in_=h[:, cc * 128:(cc + 1) * 128], identity=c_idb[:])
                dst = hTt[:, half * 8:half * 8 + 8, blk * 128:(blk + 1) * 128]
                if evq[0] % 2 == 0:
                    R.I("scalar", "activation", [p], [hTt], out=dst, in_=p[:], func=AF.Copy)
                else:
                    R.I("vector", "tensor_copy", [p], [hTt], out=dst, in_=p[:])
                evq[0] += 1
        return load_norm_transpose

    w_in_v = w_in.t.rearrange("(c p) n -> p c n", p=128)
    with ExitStack() as st:
        c_n1w = R.sb(st, "c_n1w", [128, D], F32)
        c_idf = R.sb(st, "c_idf", [128, 128], F32)
        c_idb = R.sb(st, "c_idb", [128, 128], BF16)
        c_rot = R.sb(st, "c_rot", [128, 128], F32)
        c_blk = R.sb(st, "c_blk", [128, 128], F32)
        c_qkw = R.sb(st, "c_qkw", [128, 2], F32)
        c_eps = R.sb(st, "c_eps", [128, 1], F32)
        wqk = R.sb(st, "wqk", [128, 16, 1280], BF16)
        wv = R.sb(st, "wv", [128, 16, 256], BF16)
        for dst, src in ((c_n1w, n1w), (c_idf, ident), (c_rot, rotm), (c_blk, blk1), (c_qkw, qkw)):
            R.D("sync", [src], [dst], out=dst[:], in_=src[:])
        R.I("vector", "tensor_copy", [c_idf], [c_idb], out=c_idb[:], in_=c_idf[:])
        R.I("vector", "memset", [], [c_eps], c_eps[:], EPS)
        for c0 in range(0, 16, 4):
            R.D("gpsimd", [w_in], [wqk], out=wqk[:, c0:c0 + 4, :], in_=w_in_v[:, c0:c0 + 4, 0:1280])
        R.D("gpsimd", [w_in], [wv], out=wv[:], in_=w_in_v[:, :, 1280:1536])
        lnt = mk_norm_T(st, c_n1w, c_idb, c_eps)
        hT = Rot([R.sb(st, "hT%d" % i, [128, 16, 512], BF16) for i in range(2)])
        pm = Rot([R.ps(st, "pm%d" % i, [128, 512], F32) for i in range(3)])
        p2 = Rot([R.ps(st, "p2_%d" % i, [128, 512], F32) for i in range(2)])
        cs = Rot([R.sb(st, "cs%d" % i, [128, 2, 512], F32) for i in range(2)])
        qraw = Rot([R.sb(st, "qraw%d" % i, [128, 512], F32) for i in range(2)])
        sq = Rot([R.sb(st, "sq%d" % i, [128, 512], F32) for i in range(2)])
        rr = Rot([R.sb(st, "rr%d" % i, [128, 512], F32) for i in range(2)])
        qn = Rot([R.sb(st, "qn%d" % i, [128, 512], F32) for i in range(2)])
        t1 = Rot([R.sb(st, "t1_%d" % i, [128, 512], F32) for i in range(2)])
        t2 = Rot([R.sb(st, "t2_%d" % i, [128, 512], F32) for i in range(2)])
        qf = Rot([R.sb(st, "qf%d" % i, [128, 512], BF16) for i in range(3)])
        vb = Rot([R.sb(st, "vb%d" % i, [128, 256], BF16) for i in range(2)])

        for it in range(NT + 1):
            hTt = hT.next()
            for blk in range(4):
                lnt(x_own, it * 512 + blk * 128, hTt, blk)
            cst = cs.next()
            R.D("sync", [cos_t], [cst], out=cst[:, 0, :], in_=cos_t[:, it * 512:(it + 1) * 512])
            R.D("sync", [sin_t], [cst], out=cst[:, 1, :], in_=sin_t[:, it * 512:(it + 1) * 512])
            def proj(j):
                ps = pm.next()
                for c in range(16):
                    R.I("tensor", "matmul", [wqk, hTt], [ps], ps[:], wqk[:, c, j * 128:(j + 1) * 128], hTt[:, c, :], start=(c == 0), stop=(c == 15))
                return ps
            ps_next = proj(0)
            for j in range(10):
                ps = ps_next
                qr, s_, r_, qn_, t1_, t2_, qf_ = qraw.next(), sq.next(), rr.next(), qn.next(), t1.next(), t2.next(), qf.next()
                R.I("scalar", "activation", [ps], [qr], out=qr[:], in_=ps[:], func=AF.Copy)
                R.I("scalar", "activation", [ps], [s_], out=s_[:], in_=ps[:], func=AF.Square)
                if j + 1 < 10:
                    ps_next = proj(j + 1)
                pa = p2.next()
                R.I("tensor", "matmul", [c_blk, s_], [pa], pa[:], c_blk[:], s_[:], start=True, stop=True)
                R.I("scalar", "activation", [pa, c_eps], [r_], out=r_[:], in_=pa[:], func=AF.Sqrt, bias=c_eps[:, 0:1], scale=1.0 / 64)
                R.I("vector", "reciprocal", [r_], [r_], out=r_[:], in_=r_[:])
                wcol = 0 if j < 8 else 1
                R.I("vector", "scalar_tensor_tensor", [qr, r_, c_qkw], [qn_], out=qn_[:], in0=qr[:], scalar=c_qkw[:, wcol:wcol + 1], in1=r_[:],
                    op0=ALU.mult, op1=ALU.mult)
                pb = p2.next()
                R.I("tensor", "matmul", [c_rot, qn_], [pb], pb[:], c_rot[:], qn_[:], start=True, stop=True)
                R.I("gpsimd", "tensor_tensor", [qn_, cst], [t1_], out=t1_[:], in0=qn_[:], in1=cst[:, 0, :], op=ALU.mult)
                R.I("vector", "tensor_tensor", [pb, cst], [t2_], out=t2_[:], in0=pb[:], in1=cst[:, 1, :], op=ALU.mult)
                R.I("vector", "tensor_tensor", [t1_, t2_], [qf_], out=qf_[:], in0=t1_[:], in1=t2_[:], op=ALU.add)
                if j < 8:
                    dst = Qs.t[2 * j:2 * j + 2, :, it * 512:(it + 1) * 512].rearrange("h d t -> (h d) t")
                    R.D("gpsimd", [qf_], [Qs], out=dst, in_=qf_[:])
                else:
                    dst = Ks.t[2 * (j - 8):2 * (j - 8) + 2, :, it * 512:(it + 1) * 512].rearrange("h d t -> (h d) t")
                    R.D("gpsimd", [qf_], [Ks], out=dst, in_=qf_[:])
            for blk in range(4):
                ps = pm.next()
                for c in range(16):
                    R.I("tensor", "matmul", [wv, hTt], [ps], ps[:, 0:256], hTt[:, c, blk * 128:(blk + 1) * 128], wv[:, c, :], start=(c == 0), stop=(c == 15))
                v_ = vb.next()
                R.I("scalar", "activation", [ps], [v_], out=v_[:], in_=ps[:, 0:256], func=AF.Copy)
                r0 = it * 512 + blk * 128
                R.D("gpsimd", [v_], [Vs], out=Vs.t[r0:r0 + 128, :], in_=v_[:])
        R.flush()

    UT = R.dram("UT", [3072, 2, TOK], BF16)
    if "noA2" not in cfg.debug:
      with ExitStack() as st:
        c_n1w = R.sb(st, "c_n1w", [128, D], F32)
        c_idf = R.sb(st, "c_idf", [128, 128], F32)
        c_idb = R.sb(st, "c_idb", [128, 128], BF16)
        c_eps = R.sb(st, "c_eps", [128, 1], F32)
        whx = R.sb(st, "whx", [128, 16, 3072], BF16)
        R.D("sync", [n1w], [c_n1w], out=c_n1w[:], in_=n1w[:])
        R.D("sync", [ident], [c_idf], out=c_idf[:], in_=ident[:])
        R.I("vector", "tensor_copy", [c_idf], [c_idb], out=c_idb[:], in_=c_idf[:])
        R.I("vector", "memset", [], [c_eps], c_eps[:], EPS)
        for c0 in range(0, 16, 2):
            for n0 in range(0, 3072, 1536):
                R.D("gpsimd", [w_in], [whx], out=whx[:, c0:c0 + 2, n0:n0 + 1536], in_=w_in_v[:, c0:c0 + 2, 1536 + n0:1536 + n0 + 1536])
        lnt = mk_norm_T(st, c_n1w, c_idb, c_eps)
        hT = Rot([R.sb(st, "hT%d" % i, [128, 16, 512], BF16) for i in range(2)])
        pm = Rot([R.ps(st, "pm%d" % i, [128, 512], F32) for i in range(4)])
        uo = Rot([R.sb(st, "uo%d" % i, [128, 512], BF16) for i in range(4)])
        ev = 0
        for chunk, xsrc in ((0, x_own), (1, x_oth)):
            for it in range(NT):
                hTt = hT.next()
                for blk in range(4):
                    lnt(xsrc, it * 512 + blk * 128, hTt, blk)
                for j in range(24):
                    ps = pm.next()
                    for c in range(16):
                        R.I("tensor", "matmul", [whx, hTt], [ps], ps[:], whx[:, c, j * 128:(j + 1) * 128], hTt[:, c, :], start=(c == 0), stop=(c == 15))
                    u_ = uo.next()
                    if ev % 2 == 0:
                        R.I("scalar", "activation", [ps], [u_], out=u_[:], in_=ps[:], func=AF.Copy)
                    else:
                        R.I("vector", "tensor_copy", [ps], [u_], out=u_[:], in_=ps[:])
                    ev += 1
                    R.D("gpsimd", [u_], [UT], out=UT.t[j * 128:(j + 1) * 128, chunk, it * 512:(it + 1) * 512], in_=u_[:])
        R.flush()

    if "qkv" in cfg.debug:
        with ExitStack() as st:
            dq = R.dram("dbg_q", [16, 64, TOKX], BF16, kind="ExternalOutput")
            dk = R.dram("dbg_k", [4, 64, TOKX], BF16, kind="ExternalOutput")
            dv = R.dram("dbg_v", [TOKX, 256], BF16, kind="ExternalOutput")
            tq = R.sb(st, "tq", [64, 16, TOKX], BF16)
            tk = R.sb(st, "tk", [64, 4, TOKX], BF16)
            tv = R.sb(st, "tv", [128, TOKX // 128, 256], BF16)
            R.D("sync", [Qs], [tq], out=tq[:], in_=Qs.t.rearrange("h d t -> d h t"))
            R.D("sync", [tq], [dq], out=dq.t.rearrange("h d t -> d h t"), in_=tq[:])
            R.D("sync", [Ks], [tk], out=tk[:], in_=Ks.t.rearrange("h d t -> d h t"))
            R.D("sync", [tk], [dk], out=dk.t.rearrange("h d t -> d h t"), in_=tk[:])
            R.D("sync", [Vs], [tv], out=tv[:], in_=Vs.t.rearrange("(b p) c -> p b c", p=128))
            R.D("sync", [tv], [dv], out=dv.t.rearrange("(b p) c -> p b c", p=128), in_=tv[:])
            R.flush()

    if "noH" not in cfg.debug:
      with ExitStack() as st:
        c_cw = R.sb(st, "c_cw", [128, 24, 4], F32)
        c_fl = R.sb(st, "c_fl", [128, 4], F32)
        R.D("sync", [cwt], [c_cw], out=c_cw[:], in_=cwt[:])
        R.D("sync", [eflag], [c_fl], out=c_fl[:], in_=eflag[:])
        ext = Rot([R.sb(st, "ext%d" % i, [128, TOK + 2], BF16) for i in range(2)])
        edg = Rot([R.sb(st, "edg%d" % i, [128, 2], BF16) for i in range(2)])
        PW = min(2048, TOK)
        acc = Rot([R.sb(st, "acc%d" % i, [128, PW], F32) for i in range(2)])
        ucb = Rot([R.sb(st, "ucb%d" % i, [128, TOK], BF16) for i in range(2)])
        for j in range(24):
            for chunk in range(2):
                e_ = ext.next()
                d_ = edg.next()
                R.D("sync", [UT], [e_], out=e_[:, 1:TOK + 1], in_=UT.t[j * 128:(j + 1) * 128, chunk, :])
                R.D("sync", [UT], [d_], out=d_[:, 0:1], in_=UT.t[j * 128:(j + 1) * 128, 1 - chunk, TOK - 1:TOK], allow_slow_non_contiguous=True)
                R.D("sync", [UT], [d_], out=d_[:, 1:2], in_=UT.t[j * 128:(j + 1) * 128, 1 - chunk, 0:1], allow_slow_non_contiguous=True)
                R.I("vector", "tensor_scalar", [d_, c_fl], [e_], out=e_[:, 0:1], in0=d_[:, 0:1], scalar1=c_fl[:, 2 * chunk:2 * chunk + 1], scalar2=None, op0=ALU.mult)
                R.I("vector", "tensor_scalar", [d_, c_fl], [e_], out=e_[:, TOK + 1:TOK + 2], in0=d_[:, 1:2], scalar1=c_fl[:, 2 * chunk + 1:2 * chunk + 2], scalar2=None, op0=ALU.mult)
                o_ = ucb.next()
                for p0 in range(0, TOK, PW):
                    a_ = acc.next()
                    R.I("vector", "tensor_scalar", [e_, c_cw], [a_], out=a_[:], in0=e_[:, p0:p0 + PW], scalar1=c_cw[:, j, 0:1], scalar2=c_cw[:, j, 3:4], op0=ALU.mult, op1=ALU.add)
                    R.I("vector", "scalar_tensor_tensor", [e_, c_cw, a_], [a_], out=a_[:], in0=e_[:, p0 + 1:p0 + 1 + PW], scalar=c_cw[:, j, 1:2], in1=a_[:], op0=ALU.mult, op1=ALU.add)
                    R.I("vector", "scalar_tensor_tensor", [e_, c_cw, a_], [o_], out=o_[:, p0:p0 + PW], in0=e_[:, p0 + 2:p0 + 2 + PW], scalar=c_cw[:, j, 2:3], in1=a_[:], op0=ALU.mult, op1=ALU.add)
                R.D("gpsimd", [o_], [UC], out=UC.t[j * 128:(j + 1) * 128, chunk, :], in_=o_[:])
        R.flush()

    TWO_PI = 2.0 * math.pi
    KOFF = 16.0
    if "noH" not in cfg.debug:
      with ExitStack() as st:
        c_w1 = R.sb(st, "c_w1", [33, 64], F32)
        c_w2 = R.sb(st, "c_w2", [64, 64], F32)
        c_w3 = R.sb(st, "c_w3", [64, 64], F32)
        c_w4 = R.sb(st, "c_w4", [64, 4096], F32)
        c_w4s = R.sb(st, "c_w4s", [64, 6, 2048], BF16)
        c_fv = R.sb(st, "c_fv", [64, 4], F32)
        c_sc = R.sb(st, "c_sc", [64, 8], F32)
        c_ds = R.sb(st, "c_ds", [64, 12], F32)
        c_dl = R.sb(st, "c_dl", [128, 8], F32)
        c_npi = R.sb(st, "c_npi", [64, 1], F32)
        for dst, src in ((c_w1, f_w1), (c_w2, f_w2), (c_w3, f_w3), (c_w4, f_w4), (c_fv, f_vec), (c_ds, f_dsel), (c_dl, f_delta)):
            R.D("sync", [src], [dst], out=dst[:], in_=src[:])
        R.I("vector", "memset", [], [c_npi], c_npi[:], -math.pi)
        R.I("vector", "tensor_scalar", [c_dl], [c_dl], out=c_dl[:], in0=c_dl[:], scalar1=-1.0, scalar2=None, op0=ALU.mult)
        R.I("vector", "tensor_scalar", [c_fv], [c_sc], out=c_sc[:, 0:1], in0=c_fv[:, 0:1], scalar1=1.0 / TWO_PI, scalar2=None, op0=ALU.mult)
        for l in range(1, 4):
            R.I("vector", "tensor_scalar", [c_fv, c_sc], [c_sc], out=c_sc[:, l:l + 1], in0=c_fv[:, l:l + 1], scalar1=c_sc[:, 0:1], scalar2=KOFF + 0.5, op0=ALU.mult, op1=ALU.add)
        for fh in range(6):
            R.I("vector", "tensor_scalar", [c_w4, c_ds], [c_w4s], out=c_w4s[:, fh, :], in0=c_w4[:, 0:2048], scalar1=c_ds[:, 2 * fh:2 * fh + 1], scalar2=None, op0=ALU.mult)
            R.I("vector", "scalar_tensor_tensor", [c_w4, c_ds, c_w4s], [c_w4s], out=c_w4s[:, fh, :], in0=c_w4[:, 2048:4096], scalar=c_ds[:, 2 * fh + 1:2 * fh + 2],
                in1=c_w4s[:, fh, :], op0=ALU.mult, op1=ALU.add)
        NPT = N // 512
        NF = 3 * NPT
        nacc = R.sb(st, "nacc", [128, 2, 8, NF + 3], F32)
        c_tfl = R.sb(st, "c_tfl", [128, NF + 3], F32)
        R.D("sync", [f_cnt], [c_tfl], out=c_tfl[:], in_=f_cnt[:])
        R.I("vector", "memset", [], [nacc], nacc[:], 0.0)
        zt = Rot([R.sb(st, "zt%d" % i, [33, 512], F32) for i in range(2)])
        tb = Rot([R.sb(st, "ttb%d" % i, [128, 512], F32) for i in range(3)])
        uu = Rot([R.sb(st, "uu%d" % i, [64, 512], F32) for i in range(2)])
        ki = Rot([R.sb(st, "ki%d" % i, [64, 512], I32) for i in range(2)])
        kf = Rot([R.sb(st, "kf%d" % i, [64, 512], F32) for i in range(2)])
        hh = Rot([R.sb(st, "hh%d" % i, [64, 512], F32) for i in range(4)])
        h3b = Rot([R.sb(st, "h3b%d" % i, [64, 512], BF16) for i in range(3)])
        dec = Rot([R.sb(st, "dec%d" % i, [128, 512], F32) for i in range(3)])
        tbf = Rot([R.sb(st, "tbf%d" % i, [128, 512], BF16) for i in range(6)])
        ab = Rot([R.sb(st, "ab%d" % i, [128, 512], BF16) for i in range(2)])
        ph = Rot([R.ps(st, "ph%d" % i, [64, 512], F32) for i in range(2)])
        ptp = Rot([R.ps(st, "ptp%d" % i, [128, 512], F32) for i in range(4)])

        def sin_layer(src_ps, lcol, dst):
            u_, ki_, kf_ = uu.next(), ki.next(), kf.next()
            R.I("vector", "tensor_scalar", [src_ps, c_sc], [u_], out=u_[:], in0=src_ps[:], scalar1=c_sc[:, 0:1], scalar2=c_sc[:, lcol:lcol + 1], op0=ALU.mult, op1=ALU.add)
            yield
            R.I("vector", "tensor_copy", [u_], [ki_], out=ki_[:], in_=u_[:])
            yield
            R.I("vector", "tensor_tensor", [u_, ki_], [kf_], out=kf_[:], in0=u_[:], in1=ki_[:], op=ALU.subtract)
            yield
            R.I("vector", "scalar_tensor_tensor", [kf_], [u_], out=u_[:], in0=kf_[:], scalar=0.0, in1=kf_[:], op0=ALU.is_lt, op1=ALU.add)
            R.I("scalar", "activation", [u_, c_npi], [dst], out=dst[:], in_=u_[:], func=AF.Sin, bias=c_npi[:, 0:1], scale=TWO_PI)
            yield

        def mlp_chain(f, pt_, res):
            p0 = pt_ * 512
            z_ = zt.next()
            t_ = tb.next()
            R.D("sync", [f_zt], [z_], out=z_[:], in_=f_zt.t[f, :, p0:p0 + 512])
            R.D("sync", [f_tt], [t_], out=t_[:], in_=f_tt.t[f:f + 1, p0:p0 + 512].partition_broadcast(128))
            p_ = ph.next()
            R.I("tensor", "matmul", [c_w1, z_], [p_], p_[:], c_w1[:], z_[:], start=True, stop=True)
            h1 = hh.next()
            yield from sin_layer(p_, 1, h1)
            p_ = ph.next()
            R.I("tensor", "matmul", [c_w2, h1], [p_], p_[:], c_w2[:], h1[:], start=True, stop=True)
            h2_ = hh.next()
            yield from sin_layer(p_, 2, h2_)
            p_ = ph.next()
            R.I("tensor", "matmul", [c_w3, h2_], [p_], p_[:], c_w3[:], h2_[:], start=True, stop=True)
            h3_ = h3b.next()
            yield from sin_layer(p_, 3, h3_)
            res.append((t_, h3_))

        work = [(f, pt_) for f in range(3) for pt_ in range(NPT)]
        res = []
        for _ in mlp_chain(work[0][0], work[0][1], res):
            pass
        for wi, (f, pt_) in enumerate(work):
            p0 = pt_ * 512
            half = 0 if p0 < TOK else 1
            fh = f * 2 + half
            t_, h3_ = res[wi]
            nxt = mlp_chain(work[wi + 1][0], work[wi + 1][1], res) if wi + 1 < len(work) else iter(())
            for ct in range(8):
                d_ = dec.next()
                R.I("scalar", "activation", [t_, c_dl], [d_], out=d_[:], in_=t_[:], func=AF.Exp, scale=c_dl[:, ct:ct + 1])
                for o in range(2):
                    pp_ = ptp.next()
                    R.I("tensor", "matmul", [c_w4s, h3_], [pp_], pp_[:], c_w4s[:, fh, o * 1024 + ct * 128:o * 1024 + (ct + 1) * 128], h3_[:], start=True, stop=True)
                    tb_, ab_ = tbf.next(), ab.next()
                    R.I("vector", "tensor_tensor", [pp_, d_], [tb_], out=tb_[:], in0=pp_[:], in1=d_[:], op=ALU.mult)
                    R.D("gpsimd", [tb_], [TAPS], out=TAPS.t[f, o, ct * 128:(ct + 1) * 128, p0:p0 + 512], in_=tb_[:])
                    R.I("scalar", "activation", [tb_], [ab_, nacc], out=ab_[:], in_=tb_[:], func=AF.Abs, accum_out=nacc[:, o, ct, f * NPT + pt_:f * NPT + pt_ + 1])
                    if pt_ == 0:
                        R.I("scalar", "activation", [tb_], [nacc], out=nacc[:, o, ct, NF + f:NF + f + 1], in_=tb_[:, 0:1], func=AF.Abs)
                    next(nxt, None)
            for _ in nxt:
                pass
        nrm = R.sb(st, "nrm", [128, 2, 8], F32)
        njk = R.sb(st, "njk", [128, NF + 3], F32)
        for o in range(2):
            for ct in range(8):
                R.I("vector", "scalar_tensor_tensor", [nacc, c_tfl], [njk, nrm], out=njk[:], in0=nacc[:, o, ct, :], scalar=1.0, in1=c_tfl[:], op0=ALU.mult, op1=ALU.mult,
                    accum_out=nrm[:, o, ct:ct + 1])
        R.I("vector", "reciprocal", [nrm], [nrm], out=nrm[:], in_=nrm[:])
        R.D("sync", [nrm], [RN], out=RN.t, in_=nrm[:].rearrange("p o c -> p (o c)"))
        R.flush()

    NGRP = 1024 // CG
    KP = 13 if KL % 13 == 0 else 3
    assert KL % KP == 0

    def fwd_phase(jobs):
        with ExitStack() as st:
            f1m = R.sb(st, "f1m", [128, 2 * KL], BF16)
            f2m = R.sb(st, "f2m", [128, KL, 3, 128], BF16)
            R.D("gpsimd", [m_f1], [f1m], out=f1m[0:NHI, :], in_=m_f1[:])
            for k0 in range(0, KL, 8):
                k1 = min(KL, k0 + 8)
                R.D("gpsimd", [m_f2], [f2m], out=f2m[:, k0:k1, 0:2, :], in_=m_f2[:, k0:k1, :, :])
            R.I("vector", "tensor_scalar", [f2m], [f2m], out=f2m[:, :, 2, :], in0=f2m[:, :, 1, :], scalar1=-1.0, scalar2=None, op0=ALU.mult)
            xin = Rot([R.sb(st, "xin%d" % i, [128, CG, 128], BF16) for i in range(2)])
            Ar = Rot([R.sb(st, "Afft%d" % i, [128, KL, 2, CG], BF16) for i in range(2)])
            X = Rot([R.sb(st, "Xfft%d" % i, [128, KL, 2, CG], BF16) for i in range(2)])
            Xs = R.sb(st, "Xsw", [128, KL, 2, CG], BF16)
            pF1 = Rot([R.ps(st, "pF1_%d" % i, [128, 4, 256], F32) for i in range(2)])
            pX = Rot([R.ps(st, "pX%d" % i, [128, 8, 2, CG], F32) for i in range(2)])
            ev = 0
            for (src_fn, src_obj, KIN, dst_obj, dst_fn, sw_fn) in jobs:
                for g in range(NGRP):
                    xi = xin.next()
                    A = Ar.next()
                    for cq in range(0, CG, 16):
                        R.D("sync", [src_obj], [xi], out=xi[0:KIN, cq:cq + 16, :], in_=src_fn(g)[cq:cq + 16, :].rearrange("c (h l) -> h c l", l=128))
                    for c in range(0, CG, 4):
                        p = pF1.next()
                        for i in range(4):
                            R.I("tensor", "matmul", [xi, f1m], [p], p[:, i, 0:2 * KL], xi[0:KIN, c + i, :], f1m[0:KIN, :], start=True, stop=True)
                        src = p[:, :, 0:2 * KL].rearrange("p c (r k) -> p k r c", r=2)
                        if ev % 2 == 0:
                            R.I("scalar", "activation", [p], [A], out=A[:, :, :, c:c + 4], in_=src, func=AF.Copy)
                        else:
                            R.I("vector", "tensor_copy", [p], [A], out=A[:, :, :, c:c + 4], in_=src)
                        ev += 1
                    Xt = X.next()
                    k0 = 0
                    while k0 < KL:
                        n_ = min(8, KL - k0)
                        p = pX.next()
                        for i in range(n_):
                            kk = k0 + i
                            R.I("tensor", "matmul", [f2m, A], [p], p[:, i, 0, :], f2m[:, kk, 0, :], A[:, kk, 0, :], start=True, stop=False)
                            R.I("tensor", "matmul", [f2m, A], [p], p[:, i, 0, :], f2m[:, kk, 2, :], A[:, kk, 1, :], start=False, stop=True)
                            R.I("tensor", "matmul", [f2m, A], [p], p[:, i, 1, :], f2m[:, kk, 1, :], A[:, kk, 0, :], start=True, stop=False)
                            R.I("tensor", "matmul", [f2m, A], [p], p[:, i, 1, :], f2m[:, kk, 0, :], A[:, kk, 1, :], start=False, stop=True)
                        if ev % 2 == 0:
                            R.I("scalar", "activation", [p], [Xt], out=Xt[:, k0:k0 + n_, :, :], in_=p[:, 0:n_, :, :], func=AF.Copy)
                        else:
                            R.I("vector", "tensor_copy", [p], [Xt], out=Xt[:, k0:k0 + n_, :, :], in_=p[:, 0:n_, :, :])
                        ev += 1
                        k0 += n_
                    R.D("gpsimd", [Xt], [dst_obj], out=dst_fn(g), in_=Xt[:].rearrange("p k r c -> p (k r c)"))
                    if sw_fn is not None:
                        R.I("vector", "tensor_copy", [Xt], [Xs], out=Xs[:, :, 0, :], in_=Xt[:, :, 1, :])
                        R.I("vector", "tensor_copy", [Xt], [Xs], out=Xs[:, :, 1, :], in_=Xt[:, :, 0, :])
                        R.D("gpsimd", [Xs], [GSW], out=sw_fn(g), in_=Xs[:].rearrange("p k r c -> p (k r c)"))
            R.flush()

    def inv_phase(jobs):
        with ExitStack() as st:
            i1m = R.sb(st, "i1m", [128, 2, 256], BF16)
            i2m = R.sb(st, "i2m", [128, 128, 2, NB], BF16)
            R.D("gpsimd", [m_i1], [i1m], out=i1m[:], in_=m_i1[:])
            LSTEP = max(1, 2048 // (2 * NB))
            for l0 in range(0, 128, LSTEP):
                R.D("gpsimd", [m_i2], [i2m], out=i2m[0:KL, l0:l0 + LSTEP, :, :], in_=m_i2[:, l0:l0 + LSTEP, :, :])
            idp = R.sb(st, "idp", [128, 128], BF16)
            idn = R.sb(st, "idn", [128, 128], BF16)
            R.D("gpsimd", [ident], [idp], out=idp[:], in_=ident[:])
            R.I("vector", "tensor_scalar", [idp], [idn], out=idn[:], in0=idp[:], scalar1=-1.0, scalar2=None, op0=ALU.mult)
            c_rn = R.sb(st, "c_rn", [64, 2, 8, 2], F32)
            c_nb = R.sb(st, "c_nb", [64, 2, 8, 2], F32)
            c_hb = R.sb(st, "c_hb", [64, 2, 16], F32)
            R.D("sync", [RN], [c_rn], out=c_rn[:], in_=RN.t.rearrange("(par p) (o c) -> p o c par", par=2, o=2), allow_slow_non_contiguous=True)
            R.D("sync", [hyb], [c_hb], out=c_hb[:], in_=hyb[:])
            R.I("vector", "reciprocal", [c_rn], [c_nb], out=c_nb[:], in_=c_rn[:])
            R.I("vector", "tensor_tensor", [c_nb, c_hb], [c_nb], out=c_nb[:].rearrange("p o c par -> p o (c par)"), in0=c_nb[:].rearrange("p o c par -> p o (c par)"), in1=c_hb[:], op=ALU.mult)
            pc = [Rot([R.sb(st, "pc%d_%d" % (j, i), [128, KP, 2, CG], BF16) for i in range(2)]) for j in range(6)]
            pr = [R.sb(st, "pr%d" % j, [128, KP, 2, CG], BF16) for j in range(4)]
            Y = R.sb(st, "Yfft", [128, KL, 2, CG], BF16)
            Cb = R.sb(st, "Cb", [128, 128, 2, CG], BF16)
            gt = R.sb(st, "gt", [CG, TOK], BF16)
            zt_ = R.sb(st, "zt_", [CG, TOK], BF16)
            ot = R.sb(st, "ot_", [CG, TOK], BF16)
            pYr = R.ps(st, "pYr", [128, 8, CG], F32)
            pYi = R.ps(st, "pYi", [128, 8, CG], F32)
            pC = Rot([R.ps(st, "pC%d" % i, [128, 4, 256], F32) for i in range(2)])
            pY = Rot([R.ps(st, "pY%d" % i, [CG, 8, NB], F32) for i in range(2)])
            ev = 0
            for job in jobs:
                o = job["order"]
                for g in range(NGRP):
                    rn_ap = c_rn[:, o, g // 2, (g % 2):(g % 2) + 1]
                    nb_ap = c_nb[:, o, g // 2, (g % 2):(g % 2) + 1]
                    R.D("sync", [UC], [gt], out=gt[:], in_=job["gate_fn"](g))
                    R.D("sync", [job["z_obj"]], [zt_], out=zt_[:], in_=job["z_fn"](g))
                    R.I("scalar", "activation", [gt, c_rn], [gt], out=gt[:], in_=gt[:], func=AF.Copy, scale=rn_ap)
                    R.I("vector", "tensor_scalar", [zt_, c_nb], [zt_], out=zt_[:], in0=zt_[:], scalar1=nb_ap, scalar2=None, op0=ALU.mult)
                    (Xa, Xaf, gsa), (Xb, Xbf, gsb) = job["terms"]
                    for k0 in range(0, KL, KP):
                        ks = slice(k0 * 2 * CG, (k0 + KP) * 2 * CG)
                        xa, ga, gas, xb_, gb, gbs = [pc[j].next() for j in range(6)]
                        for (t_, obj, ap_) in ((xa, Xa, Xaf(g)), (ga, GSP, GSP.t[gsa, g]), (gas, GSW, GSW.t[gsa, g]),
                                               (xb_, Xb, Xbf(g)), (gb, GSP, GSP.t[gsb, g]), (gbs, GSW, GSW.t[gsb, g])):
                            R.D("sync", [obj], [t_], out=t_[:].rearrange("p k r c -> p (k r c)"), in_=ap_[:, ks])
                        R.I("vector", "tensor_tensor", [xa, ga], [pr[0]], out=pr[0][:], in0=xa[:], in1=ga[:], op=ALU.mult)
                        R.I("vector", "tensor_tensor", [xa, gas], [pr[1]], out=pr[1][:], in0=xa[:], in1=gas[:], op=ALU.mult)
                        R.I("vector", "tensor_tensor", [xb_, gb], [pr[2]], out=pr[2][:], in0=xb_[:], in1=gb[:], op=ALU.mult)
                        R.I("vector", "tensor_tensor", [xb_, gbs], [pr[3]], out=pr[3][:], in0=xb_[:], in1=gbs[:], op=ALU.mult)
                        for j0 in range(0, KP, 8):
                            j1 = min(KP, j0 + 8)
                            nj = j1 - j0
                            R.I("tensor", "matmul", [idp, pr[0]], [pYr], pYr[:, 0:nj, :], idp[:], pr[0][:, j0:j1, 0, :], start=True, stop=False)
                            R.I("tensor", "matmul", [idn, pr[0]], [pYr], pYr[:, 0:nj, :], idn[:], pr[0][:, j0:j1, 1, :], start=False, stop=False)
                            R.I("tensor", "matmul", [idp, pr[2]], [pYr], pYr[:, 0:nj, :], idp[:], pr[2][:, j0:j1, 0, :], start=False, stop=False)
                            R.I("tensor", "matmul", [idn, pr[2]], [pYr], pYr[:, 0:nj, :], idn[:], pr[2][:, j0:j1, 1, :], start=False, stop=True)
                            R.I("tensor", "matmul", [idp, pr[1]], [pYi], pYi[:, 0:nj, :], idp[:], pr[1][:, j0:j1, 0, :], start=True, stop=False)
                            R.I("tensor", "matmul", [idp, pr[1]], [pYi], pYi[:, 0:nj, :], idp[:], pr[1][:, j0:j1, 1, :], start=False, stop=False)
                            R.I("tensor", "matmul", [idp, pr[3]], [pYi], pYi[:, 0:nj, :], idp[:], pr[3][:, j0:j1, 0, :], start=False, stop=False)
                            R.I("tensor", "matmul", [idp, pr[3]], [pYi], pYi[:, 0:nj, :], idp[:], pr[3][:, j0:j1, 1, :], start=False, stop=True)
                            R.I("scalar", "activation", [pYr], [Y], out=Y[:, k0 + j0:k0 + j1, 0, :], in_=pYr[:, 0:nj, :], func=AF.Copy)
                            R.I("scalar", "activation", [pYi], [Y], out=Y[:, k0 + j0:k0 + j1, 1, :], in_=pYi[:, 0:nj, :], func=AF.Copy)
                    for c in range(0, CG, 4):
                        p = pC.next()
                        for i in range(4):
                            R.I("tensor", "matmul", [Y, i1m], [p], p[0:KL, i, :], Y[:, :, 0, c + i], i1m[:, 0, :], start=True, stop=False)
                            R.I("tensor", "matmul", [Y, i1m], [p], p[0:KL, i, :], Y[:, :, 1, c + i], i1m[:, 1, :], start=False, stop=True)
                        src = p[0:KL, :, :].rearrange("p c (r l) -> p l r c", r=2)
                        if ev % 2 == 0:
                            R.I("scalar", "activation", [p], [Cb], out=Cb[0:KL, :, :, c:c + 4], in_=src, func=AF.Copy)
                        else:
                            R.I("vector", "tensor_copy", [p], [Cb], out=Cb[0:KL, :, :, c:c + 4], in_=src)
                        ev += 1
                    for l0 in range(0, 128, 8):
                        p = pY.next()
                        zqv = zt_[:].rearrange("c (h l) -> c l h", l=128)[:, l0:l0 + 8, :]
                        gtv = gt[:].rearrange("c (h l) -> c l h", l=128)[:, l0:l0 + 8, :]
                        otv = ot[:].rearrange("c (h l) -> c l h", l=128)[:, l0:l0 + 8, :]
                        R.I("tensor", "matmul", [idp, zt_], [p], p[:], idp[0:CG, 0:CG], zqv, start=True, stop=False)
                        for i in range(8):
                            nl = l0 + i
                            R.I("tensor", "matmul", [Cb, i2m], [p], p[:, i, :], Cb[0:KL, nl, 0, :], i2m[0:KL, nl, 0, :], start=False, stop=False)
                            R.I("tensor", "matmul", [Cb, i2m], [p], p[:, i, :], Cb[0:KL, nl, 1, :], i2m[0:KL, nl, 1, :], start=False, stop=(i == 7))
                        R.I("vector", "tensor_tensor", [p, gt], [ot], out=otv, in0=p[:], in1=gtv, op=ALU.mult)
                    R.D("gpsimd", [ot], [job["dst_obj"]], out=job["dst_fn"](g), in_=ot[:])
            R.flush()

    if "noH" not in cfg.debug:
        GIDX = {(0, 0): 0, (1, 0): 1, (2, 0): 2, (0, 1): 3, (1, 1): 4}
        jobs = []
        for (f, o), gi in GIDX.items():
            jobs.append((lambda g, f=f, o=o: TAPS.t[f, o, g * CG:(g + 1) * CG, :], TAPS, NHI, GSP, (lambda g, gi=gi: GSP.t[gi, g]), (lambda g, gi=gi: GSW.t[gi, g])))
        for ch in range(2):
            jobs.append((lambda g, ch=ch: UC.t[2048 + g * CG:2048 + (g + 1) * CG, ch, :], UC, NB, XSP, (lambda g, ch=ch: XSP.t[ch, g]), None))
        fwd_phase(jobs)
        ijobs = []
        for ch in range(2):
            gx = GIDX[(1 if ch == 0 else 2, 0)]
            ijobs.append(dict(order=0,
                              terms=[(XSP, (lambda g, ch=ch: XSP.t[ch, g]), GIDX[(0, 0)]),
                                     (XSP, (lambda g, ch=ch: XSP.t[1 - ch, g]), gx)],
                              gate_fn=(lambda g, ch=ch: UC.t[g * CG:(g + 1) * CG, ch, :]),
                              z_obj=UC, z_fn=(lambda g, ch=ch: UC.t[2048 + g * CG:2048 + (g + 1) * CG, ch, :]),
                              dst_obj=Z1, dst_fn=(lambda g, ch=ch: Z1.t[g * CG:(g + 1) * CG, ch, :])))
        inv_phase(ijobs)
        jobs = []
        for ch in range(2):
            jobs.append((lambda g, ch=ch: Z1.t[g * CG:(g + 1) * CG, ch, :], Z1, NB, XSP, (lambda g, ch=ch: XSP.t[ch, g]), None))
        fwd_phase(jobs)
        inv_phase([dict(order=1,
                        terms=[(XSP, (lambda g: XSP.t[0, g]), GIDX[(0, 1)]),
                               (XSP, (lambda g: XSP.t[1, g]), GIDX[(1, 1)])],
                        gate_fn=(lambda g: UC.t[1024 + g * CG:1024 + (g + 1) * CG, 0, :]),
                        z_obj=Z1, z_fn=(lambda g: Z1.t[g * CG:(g + 1) * CG, 0, :]),
                        dst_obj=YH, dst_fn=(lambda g: YH.t[g * CG:(g + 1) * CG, :]))])

    if "zeroYH" in cfg.debug:
        with ExitStack() as st:
            z = R.sb(st, "zt", [128, TOK], BF16)
            R.I("vector", "memset", [], [z], z[:], 0.0)
            for c in range(8):
                R.D("sync", [z], [YH], out=YH.t[c * 128:(c + 1) * 128, :], in_=z[:])
            R.flush()
    ost = ExitStack()
    rt_oh = R.sb(ost, "rt_oh", [128, NB, 2, NE], BF16)
    rt_w = R.sb(ost, "rt_w", [128, NB, 2], F32)
    rt_e = R.sb(ost, "rt_e", [128, NB, 2], F32)
    w_out_v = w_out.t.rearrange("(c p) n -> p c n", p=128)
    w_r_v = w_r.t.rearrange("(c p) n -> p c n", p=128)
    if "noB" not in cfg.debug:
      with ExitStack() as st:
        Wa = R.sb(st, "Wa", [128, 8, D], BF16)
        Wh = R.sb(st, "Wh", [128, 8, D], BF16)
        for c0 in range(0, 8, 2):
            R.D("gpsimd", [w_out], [Wa], out=Wa[:, c0:c0 + 2, :], in_=w_out_v[:, c0:c0 + 2, :])
            R.D("gpsimd", [w_out], [Wh], out=Wh[:, c0:c0 + 2, :], in_=w_out_v[:, 8 + c0:8 + c0 + 2, :])
        c_aon = R.sb(st, "c_aon", [128, 8], F32)
        c_hyn = R.sb(st, "c_hyn", [128, 8], F32)
        c_n2w = R.sb(st, "c_n2w", [128, D], F32)
        c_wr = R.sb(st, "c_wr", [128, 16, NGE], F32)
        c_br = R.sb(st, "c_br", [128, NGE], F32)
        c_iot = R.sb(st, "c_iot", [128, NE], F32)
        c_idf = R.sb(st, "c_idf", [128, 128], F32)
        c_eps = R.sb(st, "c_eps", [128, 1], F32)
        c_msk = R.sb(st, "c_msk", [128, 4, 512], BF16)
        c_snk = R.sb(st, "c_snk", [128, 8], F32)
        c_esk = R.sb(st, "c_esk", [128, 8, 128], F32)
        c_one = R.sb(st, "c_one", [128, 64], BF16)
        c_onf = R.sb(st, "c_onf", [128, 1], F32)
        for dst, src in ((c_aon, aonw), (c_hyn, hynw), (c_n2w, n2w), (c_br, b_r), (c_iot, iota_e), (c_idf, ident), (c_snk, sink2)):
            R.D("sync", [src], [dst], out=dst[:], in_=src[:])
        R.D("sync", [w_r], [c_wr], out=c_wr[:], in_=w_r_v)
        R.D("gpsimd", [masks], [c_msk], out=c_msk[:], in_=masks.t.rearrange("m p n -> p m n"))
        R.I("vector", "memset", [], [c_eps], c_eps[:], EPS)
        R.I("vector", "memset", [], [c_one], c_one[:], 1.0)
        R.I("vector", "memset", [], [c_onf], c_onf[:], 1.0)
        R.I("scalar", "activation", [c_snk], [c_snk], out=c_snk[:], in_=c_snk[:], func=AF.Exp)
        R.I("vector", "memset", [], [c_esk], c_esk[:], 0.0)
        for i in range(8):
            R.I("vector", "tensor_scalar", [c_esk, c_snk], [c_esk], out=c_esk[:, i, :], in0=c_esk[:, i, :], scalar1=c_snk[:, i:i + 1], scalar2=None, op0=ALU.add)
        for c in range(8):
            R.I("vector", "tensor_scalar", [Wa, c_aon], [Wa], out=Wa[:, c, :], in0=Wa[:, c, :], scalar1=c_aon[:, c:c + 1], scalar2=None, op0=ALU.mult)
            R.I("gpsimd", "tensor_scalar", [Wh, c_hyn], [Wh], out=Wh[:, c, :], in0=Wh[:, c, :], scalar1=c_hyn[:, c:c + 1], scalar2=None, op0=ALU.mult)

        qT = R.sb(st, "qT", [64, 16, 512], BF16)
        kT = R.sb(st, "kT", [64, 4, 768], BF16)
        Vt = R.sb(st, "Vt", [128, 6, 256], BF16)
        yh = Rot([R.sb(st, "yh%d" % i, [128, 8, 128], BF16) for i in range(2)])
        xt = Rot([R.sb(st, "xt%d" % i, [128, D], F32) for i in range(2)])
        x1 = Rot([R.sb(st, "x1_%d" % i, [128, D], F32) for i in range(1)])
        h2f = R.sb(st, "h2f", [128, D], F32)
        h2b = Rot([R.sb(st, "h2b%d" % i, [128, D], BF16) for i in range(1)])
        junk = R.sb(st, "junk", [128, D], BF16)
        h2T = R.sb(st, "h2T", [128, 16, 128], F32)
        sqa = R.sb(st, "sqa", [128, 8, 128], F32)
        yraw = R.sb(st, "yraw", [128, 2, 128], F32)
        ya = Rot([R.sb(st, "ya%d" % i, [128, 8, 128], BF16) for i in range(2)])
        sqh = R.sb(st, "sqh", [128, 8, 128], F32)
        pt = Rot([R.sb(st, "pt%d" % i, [128, 4, 128], BF16) for i in range(4)])
        dn = R.sb(st, "dn", [128, 2, 128], F32)
        sm = Rot([R.sb(st, "sm%d" % i, [128, 128], F32) for i in range(3)])
        pS = Rot([R.ps(st, "pS%d" % i, [128, 512], F32) for i in range(3)])
        pdx = R.ps(st, "pdx", [128, 512], F32)
        pden = Obj("pden_v", pdx.t[:, 0:256].rearrange("p (a b) -> p a b", a=2))
        po = R.ps(st, "po", [128, 2, 128], F32)
        ppa = R.ps(st, "ppa", [128, 512], F32)
        ppb = R.ps(st, "ppb", [128, 512], F32)
        ptr = R.ps(st, "ptr", [128, 4, 128], F32)
        psm = Obj("psm_v", pdx.t[:, 256:320])
        junk2 = R.sb(st, "junk2", [128, NE], F32)

        def router_chain(b, s_):
            R.I("vector", "tensor_reduce", [s_], [s_], out=s_[:, 5:6], in_=s_[:, 16:16 + NG], axis=AX.X, op=ALU.max)
            ohg = s_[:, 72:72 + NG]
            R.I("vector", "tensor_scalar", [s_], [s_], out=ohg, in0=s_[:, 16:16 + NG], scalar1=s_[:, 5:6], scalar2=None, op0=ALU.is_equal)
            R.I("vector", "tensor_scalar", [s_], [s_], out=s_[:, 6:7], in0=s_[:, 5:6], scalar1=-1.0, scalar2=None, op0=ALU.mult)
            R.I("scalar", "activation", [s_], [s_], out=s_[:, 76:76 + NG], in_=s_[:, 16:16 + NG], func=AF.Exp, bias=s_[:, 6:7], scale=1.0, accum_out=s_[:, 7:8])
            yield
            sel = s_[:, 80:80 + EPG]
            R.I("vector", "tensor_scalar", [s_], [s_], out=sel, in0=s_[:, 16 + NG:16 + NG + EPG], scalar1=s_[:, 72:73], scalar2=None, op0=ALU.mult)
            for gg in range(1, NG):
                R.I("vector", "scalar_tensor_tensor", [s_], [s_], out=sel, in0=s_[:, 16 + NG + gg * EPG:16 + NG + (gg + 1) * EPG], scalar=s_[:, 72 + gg:73 + gg],
                    in1=sel, op0=ALU.mult, op1=ALU.add)
            m8 = s_[:, 64:72]
            R.I("vector", "max", [s_], [s_], out=m8, in_=sel)
            yield
            R.I("vector", "tensor_scalar", [s_], [s_], out=s_[:, 6:7], in0=s_[:, 64:65], scalar1=-1.0, scalar2=None, op0=ALU.mult)
            R.I("scalar", "activation", [s_], [s_], out=s_[:, 5:6], in_=s_[:, 65:66], func=AF.Exp, bias=s_[:, 6:7], scale=1.0)
            R.I("vector", "tensor_scalar", [s_], [s_], out=s_[:, 6:7], in0=s_[:, 5:6], scalar1=1.0, scalar2=s_[:, 7:8], op0=ALU.add, op1=ALU.mult)
            R.I("vector", "reciprocal", [s_], [rt_w], out=rt_w[:, b, 0:1], in_=s_[:, 6:7])
            R.I("vector", "tensor_tensor", [s_, rt_w], [rt_w], out=rt_w[:, b, 1:2], in0=s_[:, 5:6], in1=rt_w[:, b, 0:1], op=ALU.mult)
            yield
            for k in range(2):
                ohl = s_[:, 96 + k * 8:96 + k * 8 + EPG]
                R.I("vector", "tensor_scalar", [s_], [s_], out=ohl, in0=sel, scalar1=s_[:, 64 + k:65 + k], scalar2=None, op0=ALU.is_equal)
                for gg in range(NG):
                    R.I("vector", "tensor_scalar", [s_], [rt_oh], out=rt_oh[:, b, k, gg * EPG:(gg + 1) * EPG], in0=ohl, scalar1=s_[:, 72 + gg:73 + gg],
                        scalar2=None, op0=ALU.mult)
                R.I("vector", "scalar_tensor_tensor", [rt_oh, c_iot], [junk2, rt_e], out=junk2[:, 0:NE], in0=rt_oh[:, b, k, :], scalar=1.0, in1=c_iot[:],
                    op0=ALU.mult, op1=ALU.mult, accum_out=rt_e[:, b, k:k + 1])
                yield

        pend = iter(())
        mq = [0]

        for it in range(NT):
            t0 = it * 512
            R.D("sync", [Qs], [qT], out=qT[:], in_=Qs.t[:, :, t0:t0 + 512].rearrange("h d t -> d h t"))
            R.D("sync", [Ks], [kT], out=kT[:, :, 128:640], in_=Ks.t[:, :, t0:t0 + 512].rearrange("h d t -> d h t"))
            pv0 = TOK if it == 0 else t0 - 128
            nx0 = TOK + 128 if it == NT - 1 else t0 + 512
            R.D("sync", [Ks], [kT], out=kT[:, :, 0:128], in_=Ks.t[:, :, pv0:pv0 + 128].rearrange("h d t -> d h t"))
            R.D("sync", [Ks], [kT], out=kT[:, :, 640:768], in_=Ks.t[:, :, nx0:nx0 + 128].rearrange("h d t -> d h t"))
            R.D("sync", [Vs], [Vt], out=Vt[:, 1:5, :], in_=Vs.t[t0:t0 + 512, :].rearrange("(b p) c -> p b c", p=128))
            R.D("sync", [Vs], [Vt], out=Vt[:, 0, :], in_=Vs.t[pv0:pv0 + 128, :])
            R.D("sync", [Vs], [Vt], out=Vt[:, 5, :], in_=Vs.t[nx0:nx0 + 128, :])
            for qb in range(4):
                b = it * 4 + qb
                tk0 = t0 + qb * 128
                ya_ = ya.next()
                items = [(g, kbi) for g in range(4) for kbi in range(3)]
                Sq = []

                def emit_S(g, kbi):
                    S = pS.next()
                    R.I("tensor", "matmul", [kT, qT], [S], S[:], kT[:, g, (qb + kbi) * 128:(qb + kbi + 1) * 128],
                        qT[:, 4 * g:4 * g + 4, qb * 128:(qb + 1) * 128], start=True, stop=True)
                    Sq.append(S)

                emit_S(*items[0])
                emit_S(*items[1])
                for ii, (g, kbi) in enumerate(items):
                    if ii + 2 < len(items):
                        emit_S(*items[ii + 2])
                    S = Sq[ii]
                    p_ = pt.next()
                    R.I("scalar", "activation", [S], [p_], out=p_[:], in_=S[:].rearrange("p (h q) -> p h q", h=4), func=AF.Exp, scale=0.125)
                    mi = None
                    if kbi == 0:
                        mi = 2 if b == 0 else 0
                    elif kbi == 2:
                        mi = 3 if b == NB - 1 else 1
                    if mi is not None:
                        R.I("vector", "tensor_tensor", [p_, c_msk], [p_], out=p_[:], in0=p_[:], in1=c_msk[:, mi, :].rearrange("p (h q) -> p h q", h=4), op=ALU.mult)
                    for par in range(2):
                        R.I("tensor", "matmul", [c_one, p_], [pden], pden[par * 64:(par + 1) * 64, :, :], c_one[:, 0:64], p_[:, par::2, :],
                            start=(kbi == 0), stop=(kbi == 2))
                    for par in range(2):
                        R.I("tensor", "matmul", [Vt, p_], [po], po[par * 64:(par + 1) * 64, :, :], Vt[:, qb + kbi, g * 64:(g + 1) * 64], p_[:, par::2, :],
                            start=(kbi == 0), stop=(kbi == 2))
                    if kbi == 2:
                        R.I("vector", "tensor_tensor", [pden, c_esk], [dn], out=dn[:], in0=pden[:], in1=c_esk[:, 2 * g:2 * g + 2, :], op=ALU.add)
                        R.I("vector", "reciprocal", [dn], [dn], out=dn[:], in_=dn[:])
                        R.I("vector", "tensor_tensor", [po, dn], [yraw], out=yraw[:], in0=po[:], in1=dn[:], op=ALU.mult)
                        R.I("scalar", "activation", [yraw], [sqa], out=sqa[:, 2 * g:2 * g + 2, :], in_=yraw[:], func=AF.Square)
                        R.I("scalar", "activation", [yraw], [ya_], out=ya_[:, 2 * g:2 * g + 2, :], in_=yraw[:], func=AF.Copy)
                        next(pend, None)
                yh_ = yh.next()
                R.D("sync", [YH], [yh_], out=yh_[:], in_=YH.t[:, tk0:tk0 + 128].rearrange("(c p) t -> p c t", p=128))
                R.I("scalar", "activation", [yh_], [sqh], out=sqh[:], in_=yh_[:], func=AF.Square)
                for c in range(8):
                    R.I("tensor", "matmul", [sqa, c_onf], [psm], psm[:, 0:1], sqa[:, c, :], c_onf[:, 0:1], start=(c == 0), stop=(c == 7))
                for c in range(8):
                    R.I("tensor", "matmul", [sqh, c_onf], [psm], psm[:, 1:2], sqh[:, c, :], c_onf[:, 0:1], start=(c == 0), stop=(c == 7))
                s_ = sm.next()
                R.I("scalar", "activation", [psm, c_eps], [s_], out=s_[:, 0:2], in_=psm[:, 0:2], func=AF.Sqrt, bias=c_eps[:, 0:1], scale=1.0 / 1024)
                R.I("vector", "reciprocal", [s_], [s_], out=s_[:, 0:2], in_=s_[:, 0:2])
                xt_ = xt.next()
                x1_ = x1.next()
                R.D("sync", [x_own], [xt_], out=xt_[:], in_=x_own[tk0:tk0 + 128, :])
                for ct in range(4):
                    cs_ = slice(ct * 512, (ct + 1) * 512)
                    for c in range(8):
                        R.I("tensor", "matmul", [ya_, Wa], [ppa], ppa[:], ya_[:, c, :], Wa[:, c, cs_], start=(c == 0), stop=(c == 7))
                    for c in range(8):
                        R.I("tensor", "matmul", [yh_, Wh], [ppb], ppb[:], yh_[:, c, :], Wh[:, c, cs_], start=(c == 0), stop=(c == 7))
                    R.I("vector", "scalar_tensor_tensor", [ppa, s_, xt_], [x1_], out=x1_[:, cs_], in0=ppa[:], scalar=s_[:, 0:1], in1=xt_[:, cs_], op0=ALU.mult, op1=ALU.add)
                    R.I("vector", "scalar_tensor_tensor", [ppb, s_, x1_], [x1_], out=x1_[:, cs_], in0=ppb[:], scalar=s_[:, 1:2], in1=x1_[:, cs_], op0=ALU.mult, op1=ALU.add)
                    next(pend, None)
                R.D("gpsimd", [x1_], [X1], out=X1.t[tk0:tk0 + 128, :], in_=x1_[:])
                R.I("scalar", "activation", [x1_], [junk, s_], out=junk[:], in_=x1_[:], func=AF.Square, accum_out=s_[:, 2:3])
                R.I("scalar", "activation", [s_, c_eps], [s_], out=s_[:, 3:4], in_=s_[:, 2:3], func=AF.Sqrt, bias=c_eps[:, 0:1], scale=1.0 / D)
                R.I("vector", "reciprocal", [s_], [s_], out=s_[:, 4:5], in_=s_[:, 3:4])
                R.I("vector", "scalar_tensor_tensor", [x1_, s_, c_n2w], [h2f], out=h2f[:], in0=x1_[:], scalar=s_[:, 4:5], in1=c_n2w[:], op0=ALU.mult, op1=ALU.mult)
                hb_ = h2b.next()
                R.I("scalar", "activation", [h2f], [hb_], out=hb_[:], in_=h2f[:], func=AF.Copy)
                R.D("gpsimd", [hb_], [H2], out=H2.t[tk0:tk0 + 128, :], in_=hb_[:])
                for c4 in range(4):
                    for c in range(4):
                        cc = c4 * 4 + c
                        R.I("tensor", "transpose", [h2f, c_idf], [ptr], out=ptr[:, c, :], in_=h2f[:, cc * 128:(cc + 1) * 128], identity=c_idf[:])
                    if c4 % 2 == 0:
                        R.I("scalar", "activation", [ptr], [h2T], out=h2T[:, c4 * 4:c4 * 4 + 4, :], in_=ptr[:], func=AF.Copy)
                    else:
                        R.I("vector", "tensor_copy", [ptr], [h2T], out=h2T[:, c4 * 4:c4 * 4 + 4, :], in_=ptr[:])
                for c in range(16):
                    R.I("tensor", "matmul", [h2T, c_wr], [psm], psm[:, 8:8 + NGE], h2T[:, c, :], c_wr[:, c, :], start=(c == 0), stop=(c == 15))
                lg = s_[:, 16:16 + NGE]
                R.I("vector", "tensor_tensor", [psm, c_br], [s_], out=lg, in0=psm[:, 8:8 + NGE], in1=c_br[:], op=ALU.add)
                for _ in pend:
                    pass
                pend = router_chain(b, s_)
        for _ in pend:
            pass
        R.flush()

    slots_i = R.sb(ost, "slots_i", [128, NB, 2], I32)
    if "noC" not in cfg.debug:
      with ExitStack() as st:
        c_tri = R.sb(st, "c_tri", [128, 128], F32)
        c_trb = R.sb(st, "c_trb", [128, 128], BF16)
        c_onb = R.sb(st, "c_onb", [128, 128], BF16)
        c_tok = R.sb(st, "c_tok", [128, NB], F32)
        R.D("sync", [trim], [c_tri], out=c_tri[:], in_=trim[:])
        R.D("sync", [tokid], [c_tok], out=c_tok[:], in_=tokid[:])
        R.I("vector", "tensor_copy", [c_tri], [c_trb], out=c_trb[:], in_=c_tri[:])
        R.I("vector", "memset", [], [c_onb], c_onb[:], 1.0)
        ohs = R.sb(st, "ohs", [128, NB * NE], BF16)
        R.I("vector", "tensor_tensor", [rt_oh], [ohs], out=ohs[:].rearrange("p (b e) -> p b e", e=NE), in0=rt_oh[:, :, 0, :], in1=rt_oh[:, :, 1, :], op=ALU.add)
        rk = R.sb(st, "rk", [128, NB, NE], F32)
        cumA = R.sb(st, "cumA", [128, NB, NE], F32)
        cumB = R.sb(st, "cumB", [128, NB, NE], F32)
        tt = R.sb(st, "tt", [128, NB, NE], F32)
        pp = Rot([R.ps(st, "ppd%d" % i, [128, 512], F32) for i in range(2)])
        ncol = NB * NE
        rkf = rk[:].rearrange("p b e -> p (b e)")
        ttf = tt[:].rearrange("p b e -> p (b e)")
        for c0 in range(0, ncol, 512):
            w_ = min(512, ncol - c0)
            p_ = pp.next()
            R.I("tensor", "matmul", [c_trb, ohs], [p_], p_[:, 0:w_], c_trb[:], ohs[:, c0:c0 + w_], start=True, stop=True)
            R.I("vector", "tensor_copy", [p_], [rk], out=rkf[:, c0:c0 + w_], in_=p_[:, 0:w_])
            p_ = pp.next()
            R.I("tensor", "matmul", [c_onb, ohs], [p_], p_[:, 0:w_], c_onb[:], ohs[:, c0:c0 + w_], start=True, stop=True)
            R.I("vector", "tensor_copy", [p_], [tt], out=ttf[:, c0:c0 + w_], in_=p_[:, 0:w_])
        R.I("vector", "tensor_copy", [tt], [cumA], out=cumA[:], in_=tt[:])
        cur, oth = cumA, cumB
        sft = 1
        while sft < NB:
            R.I("vector", "tensor_copy", [cur], [oth], out=oth[:, 0:sft, :], in_=cur[:, 0:sft, :])
            R.I("vector", "tensor_tensor", [cur], [oth], out=oth[:, sft:NB, :], in0=cur[:, sft:NB, :], in1=cur[:, 0:NB - sft, :], op=ALU.add)
            cur, oth = oth, cur
            sft *= 2
        R.I("vector", "tensor_tensor", [cur, tt], [oth], out=oth[:], in0=cur[:], in1=tt[:], op=ALU.subtract)
        R.I("vector", "tensor_tensor", [oth, rk], [rk], out=rk[:], in0=oth[:], in1=rk[:], op=ALU.add)
        slf = R.sb(st, "slf", [128, NB, 2], F32)
        rkk = R.sb(st, "rkk", [128, NB], F32)
        vld = R.sb(st, "vld", [128, NB], F32)
        for k in range(2):
            R.I("vector", "tensor_tensor", [rt_oh, rk], [cur], out=cur[:], in0=rt_oh[:, :, k, :], in1=rk[:], op=ALU.mult)
            R.I("vector", "tensor_reduce", [cur], [rkk], out=rkk[:], in_=cur[:], axis=AX.X, op=ALU.add)
            R.I("vector", "tensor_scalar", [rkk], [vld], out=vld[:], in0=rkk[:], scalar1=float(CAP), scalar2=None, op0=ALU.is_lt)
            R.I("vector", "scalar_tensor_tensor", [rt_e, rkk], [rkk], out=rkk[:], in0=rt_e[:, :, k], scalar=float(CAP), in1=rkk[:], op0=ALU.mult, op1=ALU.add)
            R.I("vector", "tensor_scalar", [rkk], [rkk], out=rkk[:], in0=rkk[:], scalar1=-float(NSLOT), scalar2=None, op0=ALU.add)
            R.I("vector", "tensor_tensor", [rkk, vld], [rkk], out=rkk[:], in0=rkk[:], in1=vld[:], op=ALU.mult)
            R.I("vector", "tensor_scalar", [rkk], [slf], out=slf[:, :, k], in0=rkk[:], scalar1=float(NSLOT), scalar2=None, op0=ALU.add)
        R.I("vector", "tensor_copy", [slf], [slots_i], out=slots_i[:], in_=slf[:])
        NR = (NSLOT + 128) // 128
        ini = R.sb(st, "ini", [128, NR, 2], F32)
        R.I("vector", "memset", [], [ini], ini[:, :, 0:1], float(TOK))
        R.I("vector", "memset", [], [ini], ini[:, :, 1:2], 0.0)
        R.D("sync", [ini], [SLOTI], out=SLOTI.t.rearrange("(p r) c -> p r c", p=128), in_=ini[:])
        zf = R.sb(st, "zf", [128, D], F32)
        zb = R.sb(st, "zb", [128, D], BF16)
        R.I("vector", "memset", [], [zf], zf[:], 0.0)
        R.I("vector", "memset", [], [zb], zb[:], 0.0)
        R.D("sync", [zf], [Yd], out=Yd.t[NSLOT:NSLOT + 128, :], in_=zf[:])
        R.D("sync", [zb], [H2], out=H2.t[TOK:TOK + 128, :], in_=zb[:])
        sc = R.sb(st, "sc", [128, NB, 2, 2], F32)
        for k in range(2):
            R.I("vector", "tensor_copy", [c_tok], [sc], out=sc[:, :, k, 0], in_=c_tok[:])
            R.I("vector", "tensor_copy", [rt_w], [sc], out=sc[:, :, k, 1], in_=rt_w[:, :, k])
        for b in range(NB):
            for k in range(2):
                R.dma("gpsimd", (lambda e, b=b, k=k: e.indirect_dma_start(out=SLOTI.t[:, :], out_offset=bass.IndirectOffsetOnAxis(ap=slots_i[:, b, k:k + 1], axis=0),
                                                                          in_=sc[:, b, k, :], in_offset=None)),
                      reads=[sc, slots_i, SLOTI], writes=[SLOTI], semobj=sc)
        R.flush()

    if "noD" not in cfg.debug:
      with ExitStack() as st:
        QW = min(256, DE)
        NQ = DE // QW
        DC = DE // 128
        c_idf = R.sb(st, "c_idf", [128, 128], F32)
        c_idb = R.sb(st, "c_idb", [128, 128], BF16)
        R.D("sync", [ident], [c_idf], out=c_idf[:], in_=ident[:])
        R.I("vector", "tensor_copy", [c_idf], [c_idb], out=c_idb[:], in_=c_idf[:])
        wg = Rot([R.sb(st, "wg%d" % i, [128, 16, QW], BF16) for i in range(2)])
        wu = Rot([R.sb(st, "wu%d" % i, [128, 16, QW], BF16) for i in range(2)])
        wd = Rot([R.sb(st, "wd%d" % i, [128, DC, D], BF16) for i in range(2)])
        si = Rot([R.sb(st, "si%d" % i, [128, CAPB, 2], F32) for i in range(2)])
        idx = Rot([R.sb(st, "idx%d" % i, [128, CAPB], I32) for i in range(2)])
        xg = Rot([R.sb(st, "xg%d" % i, [128, D], BF16) for i in range(3)])
        xbT = Rot([R.sb(st, "xbT%d" % i, [128, 16, CAP], BF16) for i in range(2)])
        hidT = R.sb(st, "hidT", [128, DC, CAP], BF16)
        sil = Rot([R.sb(st, "sil%d" % i, [128, 512], F32) for i in range(2)])
        yo = Rot([R.sb(st, "yo%d" % i, [128, D], F32) for i in range(2)])
        pT = Rot([R.ps(st, "pTd%d" % i, [128, 8, 128], BF16) for i in range(2)])
        pg = Rot([R.ps(st, "pg%d" % i, [128, 512], F32) for i in range(2)])
        pu = Rot([R.ps(st, "pu%d" % i, [128, 512], F32) for i in range(2)])
        pd = Rot([R.ps(st, "pd%d" % i, [128, 512], F32) for i in range(2)])
        sgs = []
        ngrp = (CAP + 511) // 512
        gsz = CAP // ngrp
        for i in range(ngrp):
            sgs.append((i * gsz, gsz))
        evq = 0
        for e_ in range(NE):
            si_ = si.next()
            idx_ = idx.next()
            R.D("sync", [SLOTI], [si_], out=si_[:], in_=SLOTI.t[e_ * CAP:(e_ + 1) * CAP, :].rearrange("(j p) c -> p j c", p=128))
            R.I("vector", "tensor_copy", [si_], [idx_], out=idx_[:], in_=si_[:, :, 0])
            xbT_ = xbT.next()
            for j in range(CAPB):
                xg_ = xg.next()
                R.dma("gpsimd", (lambda e, j=j, xg_=xg_, idx_=idx_: e.indirect_dma_start(out=xg_[:], out_offset=None, in_=H2.t[:, :],
                                                                                      in_offset=bass.IndirectOffsetOnAxis(ap=idx_[:, j:j + 1], axis=0))),
                      reads=[H2, idx_], writes=[xg_], semobj=xg_)
                for half in range(2):
                    p = pT.next()
                    for c in range(8):
                        cc = half * 8 + c
                        R.I("tensor", "transpose", [xg_, c_idb], [p], out=p[:, c, :], in_=xg_[:, cc * 128:(cc + 1) * 128], identity=c_idb[:])
                    dst = xbT_[:, half * 8:half * 8 + 8, j * 128:(j + 1) * 128]
                    if evq % 2 == 0:
                        R.I("scalar", "activation", [p], [xbT_], out=dst, in_=p[:], func=AF.Copy)
                    else:
                        R.I("vector", "tensor_copy", [p], [xbT_], out=dst, in_=p[:])
                    evq += 1
            wgv = w_gate.t[e_].rearrange("(c p) n -> p c n", p=128)
            wuv = w_up.t[e_].rearrange("(c p) n -> p c n", p=128)
            wdv = w_down.t[e_].rearrange("(c p) n -> p c n", p=128)
            wd_ = wd.next()
            for c0 in range(0, DC, 2):
                R.D("gpsimd", [w_down], [wd_], out=wd_[:, c0:c0 + 2, :], in_=wdv[:, c0:c0 + 2, :])
            for qq in range(NQ):
                wg_, wu_ = wg.next(), wu.next()
                for c0 in range(0, 16, 8):
                    R.D("gpsimd", [w_gate], [wg_], out=wg_[:, c0:c0 + 8, :], in_=wgv[:, c0:c0 + 8, qq * QW:(qq + 1) * QW])
                    R.D("gpsimd", [w_up], [wu_], out=wu_[:, c0:c0 + 8, :], in_=wuv[:, c0:c0 + 8, qq * QW:(qq + 1) * QW])
                for ct in range(QW // 128):
                    hc = qq * (QW // 128) + ct
                    for (s0, sn) in sgs:
                        pg_, pu_ = pg.next(), pu.next()
                        for c in range(16):
                            R.I("tensor", "matmul", [wg_, xbT_], [pg_], pg_[:, 0:sn], wg_[:, c, ct * 128:(ct + 1) * 128], xbT_[:, c, s0:s0 + sn], start=(c == 0), stop=(c == 15))
                        for c in range(16):
                            R.I("tensor", "matmul", [wu_, xbT_], [pu_], pu_[:, 0:sn], wu_[:, c, ct * 128:(ct + 1) * 128], xbT_[:, c, s0:s0 + sn], start=(c == 0), stop=(c == 15))
                        sl_ = sil.next()
                        R.I("scalar", "activation", [pg_], [sl_], out=sl_[:, 0:sn], in_=pg_[:, 0:sn], func=AF.Silu)
                        R.I("vector", "tensor_tensor", [sl_, pu_], [hidT], out=hidT[:, hc, s0:s0 + sn], in0=sl_[:, 0:sn], in1=pu_[:, 0:sn], op=ALU.mult)
            for sb_ in range(CAPB):
                yo_ = yo.next()
                for colt in range(4):
                    pd_ = pd.next()
                    for cc in range(DC):
                        R.I("tensor", "matmul", [hidT, wd_], [pd_], pd_[:], hidT[:, cc, sb_ * 128:(sb_ + 1) * 128], wd_[:, cc, colt * 512:(colt + 1) * 512],
                            start=(cc == 0), stop=(cc == DC - 1))
                    if colt % 2 == 0:
                        R.I("vector", "tensor_scalar", [pd_, si_], [yo_], out=yo_[:, colt * 512:(colt + 1) * 512], in0=pd_[:], scalar1=si_[:, sb_, 1:2], scalar2=None, op0=ALU.mult)
                    else:
                        R.I("scalar", "activation", [pd_, si_], [yo_], out=yo_[:, colt * 512:(colt + 1) * 512], in_=pd_[:], func=AF.Copy, scale=si_[:, sb_, 1:2])
                r0 = e_ * CAP + sb_ * 128
                R.D("sync", [yo_], [Yd], out=Yd.t[r0:r0 + 128, :], in_=yo_[:])
        R.flush()

    if "noE" not in cfg.debug:
      with ExitStack() as st:
        xa = Rot([R.sb(st, "xa%d" % i, [128, D], F32) for i in range(2)])
        g0 = Rot([R.sb(st, "g0_%d" % i, [128, D], F32) for i in range(2)])
        g1 = Rot([R.sb(st, "g1_%d" % i, [128, D], F32) for i in range(2)])
        for b in range(NB):
            xa_, g0_, g1_ = xa.next(), g0.next(), g1.next()
            R.D("sync", [X1], [xa_], out=xa_[:], in_=X1.t[b * 128:(b + 1) * 128, :])
            for k, g_ in ((0, g0_), (1, g1_)):
                R.dma("gpsimd", (lambda e, b=b, k=k, g_=g_: e.indirect_dma_start(out=g_[:], out_offset=None, in_=Yd.t[:, :],
                                                                              in_offset=bass.IndirectOffsetOnAxis(ap=slots_i[:, b, k:k + 1], axis=0))),
                      reads=[Yd, slots_i], writes=[g_], semobj=g_)
            R.I("vector", "tensor_tensor", [xa_, g0_], [xa_], out=xa_[:], in0=xa_[:], in1=g0_[:], op=ALU.add)
            R.I("vector", "tensor_tensor", [xa_, g1_], [xa_], out=xa_[:], in0=xa_[:], in1=g1_[:], op=ALU.add)
            R.D("sync", [xa_], [y_out], out=y_out.t[b * 128:(b + 1) * 128, :], in_=xa_[:])
        R.flush()

    if "H1" in cfg.debug:
        with ExitStack() as st:
            dtp = R.dram("dbg_taps", [3, 2, 1024, N], BF16, kind="ExternalOutput")
            drn = R.dram("dbg_rn", [128, 16], F32, kind="ExternalOutput")
            duc = R.dram("dbg_uc", [3072, 2, TOK], BF16, kind="ExternalOutput")
            tb_ = R.sb(st, "tbd", [128, N], BF16)
            for f in range(3):
                for o in range(2):
                    for ct in range(8):
                        R.D("sync", [TAPS], [tb_], out=tb_[:], in_=TAPS.t[f, o, ct * 128:(ct + 1) * 128, :])
                        R.D("sync", [tb_], [dtp], out=dtp.t[f, o, ct * 128:(ct + 1) * 128, :], in_=tb_[:])
            for j in range(24):
                for ch in range(2):
                    R.D("sync", [UC], [tb_], out=tb_[:, 0:TOK], in_=UC.t[j * 128:(j + 1) * 128, ch, :])
                    R.D("sync", [tb_], [duc], out=duc.t[j * 128:(j + 1) * 128, ch, :], in_=tb_[:, 0:TOK])
            tr_ = R.sb(st, "trd", [128, 16], F32)
            R.D("sync", [RN], [tr_], out=tr_[:], in_=RN.t)
            R.D("sync", [tr_], [drn], out=drn.t, in_=tr_[:])
            R.flush()
    if "B" in cfg.debug:
        with ExitStack() as st:
            dx1 = R.dram("dbg_x1", [TOK, D], F32, kind="ExternalOutput")
            drw = R.dram("dbg_rw", [128, NB, 2], F32, kind="ExternalOutput")
            dre = R.dram("dbg_re", [128, NB, 2], F32, kind="ExternalOutput")
            tb = R.sb(st, "tb", [128, D], F32)
            for b in range(NB):
                R.D("sync", [X1], [tb], out=tb[:], in_=X1.t[b * 128:(b + 1) * 128, :])
                R.D("sync", [tb], [dx1], out=dx1.t[b * 128:(b + 1) * 128, :], in_=tb[:])
            R.D("sync", [rt_w], [drw], out=drw.t, in_=rt_w[:])
            R.D("sync", [rt_e], [dre], out=dre.t, in_=rt_e[:])
            R.flush()
    ost.close()
    es.close()
    return nc


def rope_tables(pos):
    half = 8
    inv = np.power(np.float32(500000.0), -np.arange(half, dtype=np.float32) * np.float32(2.0) / np.float32(16.0)).astype(np.float32)
    ang = pos.astype(np.float32)[None, :] * inv[:, None]
    c = np.cos(ang).astype(np.float32)
    s = np.sin(ang).astype(np.float32)
    n = pos.shape[0]
    C = np.ones((64, n), np.float32)
    S = np.zeros((64, n), np.float32)
    C[0:8] = c
    C[8:16] = c
    S[0:8] = -s
    S[8:16] = s
    return np.concatenate([C, C], 0), np.concatenate([S, S], 0)


def const_tables():
    ident = np.eye(128, dtype=np.float32)
    rot = np.zeros((128, 128), np.float32)
    for m in range(128):
        d = m % 64
        if d < 8:
            rot[m + 8, m] = 1.0
        elif d < 16:
            rot[m - 8, m] = 1.0
    blk = np.zeros((128, 128), np.float32)
    blk[0:64, 0:64] = 1.0
    blk[64:128, 64:128] = 1.0
    return ident, rot, blk


def fft_tables(TOK):
    N = 2 * TOK
    NHI = N // 128
    KL = NHI // 2 + 1
    NB = TOK // 128
    f64 = np.float64
    nh = np.arange(NHI, dtype=f64)[:, None]
    kl = np.arange(KL, dtype=f64)[None, :]
    th = 2 * np.pi * nh * kl / NHI
    m_f1 = np.concatenate([np.cos(th), -np.sin(th)], 1).astype(np.float32)
    nl = np.arange(128, dtype=f64)[:, None, None]
    klo = np.arange(KL, dtype=f64)[None, :, None]
    kh = np.arange(128, dtype=f64)[None, None, :]
    th = 2 * np.pi * (nl * klo / N + nl * kh / 128.0)
    m_f2 = np.stack([np.cos(th), -np.sin(th)], 2).astype(np.float32)
    khh = np.arange(128, dtype=f64)[:, None]
    nll = np.arange(128, dtype=f64)[None, :]
    th = 2 * np.pi * khh * nll / 128.0
    cr, ci = np.cos(th), np.sin(th)
    m_i1 = np.stack([np.concatenate([cr, ci], 1), np.concatenate([-ci, cr], 1)], 1).astype(np.float32)
    wgt = np.full((KL,), 2.0)
    wgt[0] = 1.0
    wgt[KL - 1] = 1.0
    klo = np.arange(KL, dtype=f64)[:, None, None]
    nl = np.arange(128, dtype=f64)[None, :, None]
    nhi = np.arange(NB, dtype=f64)[None, None, :]
    th = 2 * np.pi * (nl * klo / N + nhi * klo / NHI)
    sc = (wgt / N)[:, None, None]
    m_i2 = np.stack([sc * np.cos(th), -sc * np.sin(th)], 2).astype(np.float32)
    return (np.ascontiguousarray(m_f1), np.ascontiguousarray(m_f2), np.ascontiguousarray(m_i1), np.ascontiguousarray(m_i2))


def filter_tables(TOK, L, h):
    f32 = np.float32
    N = 2 * TOK
    n = np.arange(N)
    t_lin = np.linspace(0.0, 1.0, L, dtype=f32)
    fr = np.linspace(1e-4, 15.0, 16, dtype=f32)

    def feats(lags):
        t = t_lin[lags]
        w = (f32(2.0 * math.pi) * lags.astype(f32) / f32(L)).astype(f32)
        ang = (w[:, None] * fr[None, :]).astype(f32)
        return np.concatenate([t[:, None], np.cos(ang), -np.sin(ang)], 1).astype(f32).T

    zt = np.zeros((3, 33, N), f32)
    tt = np.full((3, N), 1.0e4, f32)
    cnt = np.zeros((3, N), f32)
    dsel = np.zeros((3, 2, 2), f32)
    dsel[:, :, 0] = 1.0
    lag = np.where(n < TOK, n, N - n)
    lag[TOK] = 0
    valid = n != TOK
    zt[0] = feats(lag)
    tt[0][valid] = t_lin[lag[valid]]
    cnt[0][valid] = 1.0
    dsel[0, 0] = (1.0, 0.0)
    dsel[0, 1] = (0.0, 1.0)
    if L == 2 * TOK:
        lag_f = np.where(n < TOK, TOK + n, n - TOK)
        lag_f[TOK] = 0
        cnt_f = ((n < TOK)).astype(f32)
        lag_b = np.where(n < TOK, TOK - n, N + TOK - n)
        lag_b[TOK] = 0
        cnt_b = ((n == 0) | (n > TOK)).astype(f32)
        kinds = ("b", "f") if h == 0 else ("f", "b")
        for fi, kd in zip((1, 2), kinds):
            lg = lag_f if kd == "f" else lag_b
            zt[fi] = feats(lg)
            tt[fi][valid] = t_lin[lg[valid]]
            cnt[fi] = cnt_f if kd == "f" else cnt_b
            cnt[fi][TOK] = 0.0
            dsel[fi, :, :] = (1.0, 0.0) if kd == "f" else (0.0, 1.0)
    NPT = N // 512
    tfl = np.zeros((3 * NPT + 3,), f32)
    tfl[0:NPT] = 1.0
    if L == 2 * TOK:
        kinds = ("b", "f") if h == 0 else ("f", "b")
        for fi, kd in zip((1, 2), kinds):
            for pt in range(NPT):
                if kd == "f":
                    tfl[fi * NPT + pt] = 1.0 if pt * 512 < TOK else 0.0
                else:
                    tfl[fi * NPT + pt] = 1.0 if pt * 512 >= TOK else 0.0
            if kd == "b":
                tfl[3 * NPT + fi] = 1.0
    cnt = np.ascontiguousarray(np.broadcast_to(tfl[None, :], (128, 3 * NPT + 3)))
    return zt, tt, cnt, dsel


def prep(cfg, inp):
    TOK, TOKX, D = cfg.TOK, cfg.TOKX, cfg.D
    ident, rot, blk = const_tables()
    f32 = np.float32
    xp = np.asarray(inp["x_prompt"], f32)
    xs = np.asarray(inp["x_sample"], f32)
    w_in = np.ascontiguousarray(np.asarray(inp["w_in"], f32)[0])
    n1w = np.ascontiguousarray(np.broadcast_to(np.asarray(inp["norm1_w"], f32)[0][None, :], (128, D)))
    qkw = np.stack([np.tile(np.asarray(inp["q_norm_w"], f32)[0], 2), np.tile(np.asarray(inp["k_norm_w"], f32)[0], 2)], 1)
    NG, NE, EPG = cfg.NG, cfg.NE, cfg.EPG
    w_out = np.ascontiguousarray(np.asarray(inp["w_out"], f32)[0])
    aon = np.ascontiguousarray(np.asarray(inp["attn_out_norm_w"], f32)[0].reshape(8, 128).T)
    hyn = np.ascontiguousarray(np.asarray(inp["hy_out_norm_w"], f32)[0].reshape(8, 128).T)
    n2w = np.ascontiguousarray(np.broadcast_to(np.asarray(inp["norm2_w"], f32)[0][None, :], (128, D)))
    w_r = np.ascontiguousarray(np.concatenate([np.asarray(inp["w_route_group"], f32)[0], np.asarray(inp["w_route_expert"], f32)[0]], 1))
    b_r = np.concatenate([np.asarray(inp["b_route_group"], f32)[0], np.asarray(inp["b_route_expert"], f32)[0]])
    b_r = np.ascontiguousarray(np.broadcast_to(b_r[None, :], (128, NG + NE)))
    iota_e = np.ascontiguousarray(np.broadcast_to(np.arange(NE, dtype=f32)[None, :], (128, NE)))
    sink = np.asarray(inp["attn_sink"], f32)[0]
    sink2 = np.zeros((128, 8), f32)
    for g in range(4):
        for j in range(2):
            sink2[:64, g * 2 + j] = sink[4 * g + 2 * j]
            sink2[64:, g * 2 + j] = sink[4 * g + 2 * j + 1]
    m_f1, m_f2, m_i1, m_i2 = fft_tables(TOK)
    cw = np.asarray(inp["conv_w"], f32)[0]
    cb = np.asarray(inp["conv_b"], f32)[0]
    cwt = np.ascontiguousarray(np.stack([cw[0], cw[1], cw[2], cb], 1).reshape(24, 128, 4).transpose(1, 0, 2))
    f_vec = np.ascontiguousarray(np.stack([np.asarray(inp[k], f32)[0] for k in ("filt_freq", "filt_b1", "filt_b2", "filt_b3")], 1))
    max_decay = math.log(1e-2) / 0.3
    min_decay = math.log(1e-2) / 1.5
    deltas = np.abs(np.linspace(min_decay, max_decay, 1024, dtype=f32))
    f_delta = np.ascontiguousarray(deltas.reshape(8, 128).T)
    hyb = np.ascontiguousarray(np.asarray(inp["hy_bias"], f32)[0].reshape(2, 16, 64).transpose(2, 0, 1))
    trim = (np.arange(128)[:, None] < np.arange(128)[None, :]).astype(f32)
    tokid = (np.arange(128)[:, None] + 128 * np.arange(cfg.NB)[None, :]).astype(f32)
    w_gate = np.ascontiguousarray(np.asarray(inp["w_gate"], f32)[0])
    w_up = np.ascontiguousarray(np.asarray(inp["w_up"], f32)[0])
    w_down = np.ascontiguousarray(np.asarray(inp["w_down"], f32)[0])
    jj = np.arange(128)[:, None]
    qi = np.arange(128)[None, :]
    tri_prev = np.tile((jj >= qi).astype(f32), (1, 4))
    tri_next = np.tile((jj <= qi).astype(f32), (1, 4))
    maps = []
    for c in range(8):
        if c < 4:
            seq, h, L = xp[c], 0, TOK
        else:
            seq, h, L = xs[(c - 4) // 2], (c - 4) % 2, 2 * TOK
        base = h * TOK
        x_own = np.zeros((TOKX, D), f32)
        x_own[:TOK] = seq[base:base + TOK]
        if base >= 128:
            x_own[TOK:TOK + 128] = seq[base - 128:base]
        if base + TOK + 128 <= L:
            x_own[TOK + 128:TOK + 256] = seq[base + TOK:base + TOK + 128]
        x_oth = np.zeros((TOK, D), f32)
        if L > TOK:
            x_oth[:] = seq[(1 - h) * TOK:(2 - h) * TOK]
        pos = np.concatenate([base + np.arange(TOK), base - 128 + np.arange(128), base + TOK + np.arange(128), np.zeros(256)]).astype(f32)
        ct, stb = rope_tables(pos)
        has_prev = base >= 128
        has_next = base + TOK + 128 <= L
        mk = np.stack([tri_prev, tri_next, tri_prev * (1.0 if has_prev else 0.0), tri_next * (1.0 if has_next else 0.0)]).astype(f32)
        zt_, tt_, cnt_, dsel_ = filter_tables(TOK, L, h)
        two = (L == 2 * TOK)
        ef = np.array([1.0 if h == 1 else 0.0, 1.0 if (two and h == 0) else 0.0, 1.0 if (two and h == 0) else 0.0, 1.0 if h == 1 else 0.0], f32)
        m = dict(cwt=cwt, eflag=np.ascontiguousarray(np.broadcast_to(ef[None, :], (128, 4))), f_w1=np.ascontiguousarray(np.asarray(inp["filt_w1"], f32)[0]),
                 f_w2=np.ascontiguousarray(np.asarray(inp["filt_w2"], f32)[0]), f_w3=np.ascontiguousarray(np.asarray(inp["filt_w3"], f32)[0]),
                 f_w4=np.ascontiguousarray(np.asarray(inp["filt_w4"], f32)[0]), f_vec=f_vec, f_zt=zt_, f_tt=tt_, f_cnt=cnt_,
                 f_dsel=np.ascontiguousarray(np.broadcast_to(dsel_.reshape(1, 12), (64, 12))), f_delta=f_delta, hyb=hyb,
                 m_f1=m_f1, m_f2=m_f2, m_i1=m_i1, m_i2=m_i2, trim=trim, tokid=tokid, w_gate=w_gate, w_up=w_up, w_down=w_down, masks=mk, sink2=sink2, w_out=w_out, aonw=aon, hynw=hyn, n2w=n2w, w_r=w_r, b_r=b_r, iota_e=iota_e, x_own=x_own, x_oth=x_oth, w_in=w_in, n1w=n1w, ident=ident, rotm=rot, blk1=blk, qkw=np.ascontiguousarray(qkw),
                 cos_t=np.ascontiguousarray(ct), sin_t=np.ascontiguousarray(stb))
        maps.append(m)
    return maps


_CACHE = {}


def kernel(**inputs):
    cfg = Cfg()
    if "nc" not in _CACHE:
        _CACHE["nc"] = build(cfg)
    nc = _CACHE["nc"]
    maps = prep(cfg, inputs)
    res = run_bass_kernel_spmd(nc, maps, core_ids=list(range(8)))
    outs = [np.asarray(r["y"], np.float32) for r in res.results]
    TOK = cfg.TOK
    y_prompt = np.stack(outs[0:4], 0)
    y_sample = np.stack([np.concatenate([outs[4], outs[5]], 0), np.concatenate([outs[6], outs[7]], 0)], 0)
    return (y_prompt, y_sample)
```

```python
import math
from contextlib import ExitStack
import numpy as np
import concourse.bass as bass
import concourse.mybir as mybir
from concourse.bass_utils import run_bass_kernel_spmd

F32 = mybir.dt.float32
BF16 = mybir.dt.bfloat16
I32 = mybir.dt.int32
U32 = mybir.dt.uint32
AF = mybir.ActivationFunctionType
ALU = mybir.AluOpType
AX = mybir.AxisListType

ENGS = ("tensor", "vector", "scalar", "gpsimd", "sync")
SAME_ENGINE_SYNC = True
EPS = 1e-6


class Obj:
    __slots__ = ("name", "t", "w", "r", "sem", "is_dram")

    def __init__(self, name, t=None, is_dram=False):
        self.name = name
        self.t = t
        self.w = {}
        self.r = {}
        self.sem = None
        self.is_dram = is_dram

    def __getitem__(self, idx):
        return self.t[idx]


class Rec:
    def __init__(self, nc, es, n_dma_sems=92):
        self.nc = nc
        self.sems = {}
        self.val = {}
        for e in ENGS:
            self.sems[e] = es.enter_context(nc.semaphore("S_" + e))
            self.val[e] = 0
        self.free_dma = {"hw": [], "sw": []}
        for i in range(n_dma_sems):
            k = "D%d" % i
            self.sems[k] = es.enter_context(nc.semaphore(k))
            self.val[k] = 0
            self.free_dma["hw" if i % 2 == 0 else "sw"].append(k)
        self.phase_dma = []
        self.ops = {e: [] for e in ENGS}
        self.seen = {e: {} for e in ENGS}
        self.nops = 0
        self.sem_sw = {}
        self.uid = 0

    def sb(self, st, name, shape, dt):
        self.uid += 1
        name = "%s_u%d" % (name, self.uid)
        return Obj(name, st.enter_context(self.nc.sbuf_tensor(name, list(shape), dt)))

    def ps(self, st, name, shape, dt):
        self.uid += 1
        name = "%s_u%d" % (name, self.uid)
        return Obj(name, st.enter_context(self.nc.psum_tensor(name, list(shape), dt)))

    def dram(self, name, shape, dt, kind="Internal"):
        t = self.nc.dram_tensor(name, list(shape), dt, kind=kind)
        return Obj(name, t.ap(), is_dram=True)

    def _dsem(self, o, kind):
        if o.sem is None:
            o.sem = {}
        if kind not in o.sem:
            o.sem[kind] = self.free_dma[kind].pop()
            self.phase_dma.append((o, kind))
        return o.sem[kind]

    def _deps(self, eng, reads, writes):
        need = {}
        for o in reads:
            for k, v in o.w.items():
                if need.get(k, 0) < v:
                    need[k] = v
        for o in writes:
            for d in (o.w, o.r):
                for k, v in d.items():
                    if need.get(k, 0) < v:
                        need[k] = v
        waits = []
        seen = self.seen[eng]
        for k, v in need.items():
            if k == eng and (eng == "tensor" or not SAME_ENGINE_SYNC):
                continue
            if seen.get(k, 0) >= v:
                continue
            seen[k] = v
            waits.append((k, v))
        return waits

    def _mark(self, reads, writes, key, value):
        for o in reads:
            if o.r.get(key, 0) < value:
                o.r[key] = value
        for o in writes:
            o.w = {key: value}
            o.r = {}

    def op(self, eng, fn, reads=(), writes=()):
        waits = self._deps(eng, reads, writes)
        self.val[eng] += 1
        self.ops[eng].append((waits, fn, eng, 1))
        self._mark(reads, writes, eng, self.val[eng])
        self.nops += 1

    def dma(self, q, fn, reads=(), writes=(), semobj=None):
        if semobj is None:
            semobj = [o for o in list(writes) + list(reads) if not o.is_dram][0]
        key = self._dsem(semobj, "sw" if q == "gpsimd" else "hw")
        waits = self._deps(q, reads, writes)
        if q == "gpsimd" and self.val[key] > 0 and self.seen[q].get(key, 0) < self.val[key]:
            self.seen[q][key] = self.val[key]
            waits.append((key, self.val[key]))
        self.val[key] += 16
        self.ops[q].append((waits, fn, key, 16))
        self._mark(reads, writes, key, self.val[key])
        self.nops += 1

    def I(self, eng, meth, reads, writes, *a, **kw):
        self.op(eng, lambda e: getattr(e, meth)(*a, **kw), reads, writes)

    def D(self, q, reads, writes, semobj=None, **kw):
        self.dma(q, lambda e: e.dma_start(**kw), reads, writes, semobj)

    def flush(self):
        for e in ENGS:
            waits = []
            for k, v in self.val.items():
                if v > 0 and self.seen[e].get(k, 0) < v and k != e:
                    self.seen[e][k] = v
                    waits.append((k, v))
            self.ops[e].append((waits, None, None, 0))
        sems = self.sems
        ops = self.ops
        with self.nc.Block() as block:
            def mk(ename):
                def body(e):
                    for waits, fn, key, inc in ops[ename]:
                        for k, v in waits:
                            e.wait_ge(sems[k], v)
                        if fn is not None:
                            fn(e).then_inc(sems[key], inc)
                return body
            block.tensor(mk("tensor"))
            block.vector(mk("vector"))
            block.scalar(mk("scalar"))
            block.gpsimd(mk("gpsimd"))
            block.sync(mk("sync"))
        self.ops = {e: [] for e in ENGS}
        for o, kind in self.phase_dma:
            self.free_dma[kind].append(o.sem.pop(kind))
        self.phase_dma = []


class Rot:
    def __init__(self, items):
        self.items = items
        self.i = 0

    def next(self):
        o = self.items[self.i % len(self.items)]
        self.i += 1
        return o


class Cfg:
    def __init__(self, TOK=8192, NE=32, EPG=8, DE=1024, CAP=640, debug=()):
        self.TOK = TOK
        self.NB = TOK // 128
        self.NT = TOK // 512
        self.TOKX = TOK + 512
        self.D = 2048
        self.NE = NE
        self.EPG = EPG
        self.NG = NE // EPG
        self.DE = DE
        self.CAP = CAP
        self.NSLOT = NE * CAP
        self.debug = tuple(debug)


def build(cfg):
    nc = bass.Bass("TRN2", target_bir_lowering=False)
    TOK, NB, NT, TOKX, D = cfg.TOK, cfg.NB, cfg.NT, cfg.TOKX, cfg.D
    es = ExitStack()
    R = Rec(nc, es)

    def ein(name, shape, dt=F32):
        return R.dram(name, shape, dt, kind="ExternalInput")

    x_own = ein("x_own", [TOKX, D])
    x_oth = ein("x_oth", [TOK, D])
    w_in = ein("w_in", [D, 4608])
    n1w = ein("n1w", [128, D])
    ident = ein("ident", [128, 128])
    rotm = ein("rotm", [128, 128])
    blk1 = ein("blk1", [128, 128])
    qkw = ein("qkw", [128, 2])
    cos_t = ein("cos_t", [128, TOKX])
    sin_t = ein("sin_t", [128, TOKX])
    NG, NE, EPG = cfg.NG, cfg.NE, cfg.EPG
    NGE = NG + NE
    masks = ein("masks", [4, 128, 512])
    sink2 = ein("sink2", [128, 8])
    w_out = ein("w_out", [D, D])
    aonw = ein("aonw", [128, 8])
    hynw = ein("hynw", [128, 8])
    n2w = ein("n2w", [128, D])
    w_r = ein("w_r", [D, NGE])
    b_r = ein("b_r", [128, NGE])
    iota_e = ein("iota_e", [128, NE])
    DE, CAP, NSLOT = cfg.DE, cfg.CAP, cfg.NSLOT
    CAPB = CAP // 128
    w_gate = ein("w_gate", [NE, D, DE])
    w_up = ein("w_up", [NE, D, DE])
    w_down = ein("w_down", [NE, DE, D])
    trim = ein("trim", [128, 128])
    tokid = ein("tokid", [128, NB])
    y_out = R.dram("y", [TOK, D], F32, kind="ExternalOutput")
    SLOTI = R.dram("SLOTI", [NSLOT + 128, 2], F32)
    Yd = R.dram("Yd", [NSLOT + 128, D], F32)
    N = 2 * TOK
    NHI = N // 128
    KL = NHI // 2 + 1
    CG = 64
    cwt = ein("cwt", [128, 24, 4])
    eflag = ein("eflag", [128, 4])
    f_w1 = ein("f_w1", [33, 64])
    f_w2 = ein("f_w2", [64, 64])
    f_w3 = ein("f_w3", [64, 64])
    f_w4 = ein("f_w4", [64, 4096])
    f_vec = ein("f_vec", [64, 4])
    f_zt = ein("f_zt", [3, 33, N])
    f_tt = ein("f_tt", [3, N])
    f_cnt = ein("f_cnt", [128, 3 * (N // 512) + 3])
    f_dsel = ein("f_dsel", [64, 12])
    f_delta = ein("f_delta", [128, 8])
    hyb = ein("hyb", [64, 2, 16])
    m_f1 = ein("m_f1", [NHI, 2 * KL])
    m_f2 = ein("m_f2", [128, KL, 2, 128])
    m_i1 = ein("m_i1", [128, 2, 256])
    m_i2 = ein("m_i2", [KL, 128, 2, NB])
    UC = R.dram("UC", [3072, 2, TOK], BF16)
    TAPS = R.dram("TAPS", [3, 2, 1024, N], BF16)
    RN = R.dram("RN", [128, 16], F32)
    GSP = R.dram("GSP", [5, 16, 128, KL * 2 * CG], BF16)
    GSW = R.dram("GSW", [5, 16, 128, KL * 2 * CG], BF16)
    XSP = R.dram("XSP", [2, 16, 128, KL * 2 * CG], BF16)
    Z1 = R.dram("Z1", [1024, 2, TOK], BF16)
    YH = R.dram("YH", [1024, TOK], BF16)
    X1 = R.dram("X1", [TOK, D], F32)
    H2 = R.dram("H2", [TOK + 128, D], BF16)
    Qs = R.dram("Qs", [16, 64, TOKX], BF16)
    Ks = R.dram("Ks", [4, 64, TOKX], BF16)
    Vs = R.dram("Vs", [TOKX, 256], BF16)
    dbg = {}

    def mk_norm_T(st, c_n1w, c_idb, c_eps):
        xb = Rot([R.sb(st, "xb%d" % i, [128, D], F32) for i in range(3)])
        junk = R.sb(st, "junk", [128, D], BF16)
        hb = Rot([R.sb(st, "hb%d" % i, [128, D], BF16) for i in range(4)])
        stat = Rot([R.sb(st, "stat%d" % i, [128, 4], F32) for i in range(4)])
        pT = Rot([R.ps(st, "pT%d" % i, [128, 8, 128], BF16) for i in range(2)])
        evq = [0]

        def load_norm_transpose(xsrc, row0, hTt, blk):
            xt = xb.next()
            R.D("sync", [xsrc], [xt], out=xt[:], in_=xsrc[row0:row0 + 128, :])
            s = stat.next()
            R.I("scalar", "activation", [xt], [junk, s], out=junk[:], in_=xt[:], func=AF.Square, accum_out=s[:, 0:1])
            R.I("scalar", "activation", [s, c_eps], [s], out=s[:, 1:2], in_=s[:, 0:1], func=AF.Sqrt, bias=c_eps[:, 0:1], scale=1.0 / D)
            R.I("vector", "reciprocal", [s], [s], out=s[:, 2:3], in_=s[:, 1:2])
            h = hb.next()
            R.I("vector", "scalar_tensor_tensor", [xt, s, c_n1w], [h], out=h[:], in0=xt[:], scalar=s[:, 2:3], in1=c_n1w[:],
                op0=ALU.mult, op1=ALU.mult)
            for half in range(2):
                p = pT.next()
                for c in range(8):
                    cc = half * 8 + c
                    R.I("tensor", "transpose", [h, c_idb], [p], out=p[:, c, :], in_=h[:, cc * 128:(cc + 1) * 128], identity=c_idb[:])
                dst = hTt[:, half * 8:half * 8 + 8, blk * 128:(blk + 1) * 128]
                if evq[0] % 2 == 0:
                    R.I("scalar", "activation", [p], [hTt], out=dst, in_=p[:], func=AF.Copy)
                else:
                    R.I("vector", "tensor_copy", [p], [hTt], out=dst, in_=p[:])
                evq[0] += 1
        return load_norm_transpose

    w_in_v = w_in.t.rearrange("(c p) n -> p c n", p=128)
    with ExitStack() as st:
        c_n1w = R.sb(st, "c_n1w", [128, D], F32)
        c_idf = R.sb(st, "c_idf", [128, 128], F32)
        c_idb = R.sb(st, "c_idb", [128, 128], BF16)
        c_rot = R.sb(st, "c_rot", [128, 128], F32)
        c_blk = R.sb(st, "c_blk", [128, 128], F32)
        c_qkw = R.sb(st, "c_qkw", [128, 2], F32)
        c_eps = R.sb(st, "c_eps", [128, 1], F32)
        wqk = R.sb(st, "wqk", [128, 16, 1280], BF16)
        wv = R.sb(st, "wv", [128, 16, 256], BF16)
        for dst, src in ((c_n1w, n1w), (c_idf, ident), (c_rot, rotm), (c_blk, blk1), (c_qkw, qkw)):
            R.D("sync", [src], [dst], out=dst[:], in_=src[:])
        R.I("vector", "tensor_copy", [c_idf], [c_idb], out=c_idb[:], in_=c_idf[:])
        R.I("vector", "memset", [], [c_eps], c_eps[:], EPS)
        for c0 in range(0, 16, 4):
            R.D("gpsimd", [w_in], [wqk], out=wqk[:, c0:c0 + 4, :], in_=w_in_v[:, c0:c0 + 4, 0:1280])
        R.D("gpsimd", [w_in], [wv], out=wv[:], in_=w_in_v[:, :, 1280:1536])
        lnt = mk_norm_T(st, c_n1w, c_idb, c_eps)
        hT = Rot([R.sb(st, "hT%d" % i, [128, 16, 512], BF16) for i in range(2)])
        pm = Rot([R.ps(st, "pm%d" % i, [128, 512], F32) for i in range(3)])
        p2 = Rot([R.ps(st, "p2_%d" % i, [128, 512], F32) for i in range(2)])
        cs = Rot([R.sb(st, "cs%d" % i, [128, 2, 512], F32) for i in range(2)])
        qraw = Rot([R.sb(st, "qraw%d" % i, [128, 512], F32) for i in range(2)])
        sq = Rot([R.sb(st, "sq%d" % i, [128, 512], F32) for i in range(2)])
        rr = Rot([R.sb(st, "rr%d" % i, [128, 512], F32) for i in range(2)])
        qn = Rot([R.sb(st, "qn%d" % i, [128, 512], F32) for i in range(2)])
        t1 = Rot([R.sb(st, "t1_%d" % i, [128, 512], F32) for i in range(2)])
        t2 = Rot([R.sb(st, "t2_%d" % i, [128, 512], F32) for i in range(2)])
        qf = Rot([R.sb(st, "qf%d" % i, [128, 512], BF16) for i in range(3)])
        vb = Rot([R.sb(st, "vb%d" % i, [128, 256], BF16) for i in range(2)])

        for it in range(NT + 1):
            hTt = hT.next()
            for blk in range(4):
                lnt(x_own, it * 512 + blk * 128, hTt, blk)
            cst = cs.next()
            R.D("sync", [cos_t], [cst], out=cst[:, 0, :], in_=cos_t[:, it * 512:(it + 1) * 512])
            R.D("sync", [sin_t], [cst], out=cst[:, 1, :], in_=sin_t[:, it * 512:(it + 1) * 512])
            def proj(j):
                ps = pm.next()
                for c in range(16):
                    R.I("tensor", "matmul", [wqk, hTt], [ps], ps[:], wqk[:, c, j * 128:(j + 1) * 128], hTt[:, c, :], start=(c == 0), stop=(c == 15))
                return ps
            ps_next = proj(0)
            for j in range(10):
                ps = ps_next
                qr, s_, r_, qn_, t1_, t2_, qf_ = qraw.next(), sq.next(), rr.next(), qn.next(), t1.next(), t2.next(), qf.next()
                R.I("scalar", "activation", [ps], [qr], out=qr[:], in_=ps[:], func=AF.Copy)
                R.I("scalar", "activation", [ps], [s_], out=s_[:], in_=ps[:], func=AF.Square)
                if j + 1 < 10:
                    ps_next = proj(j + 1)
                pa = p2.next()
                R.I("tensor", "matmul", [c_blk, s_], [pa], pa[:], c_blk[:], s_[:], start=True, stop=True)
                R.I("scalar", "activation", [pa, c_eps], [r_], out=r_[:], in_=pa[:], func=AF.Sqrt, bias=c_eps[:, 0:1], scale=1.0 / 64)
                R.I("vector", "reciprocal", [r_], [r_], out=r_[:], in_=r_[:])
                wcol = 0 if j < 8 else 1
                R.I("vector", "scalar_tensor_tensor", [qr, r_, c_qkw], [qn_], out=qn_[:], in0=qr[:], scalar=c_qkw[:, wcol:wcol + 1], in1=r_[:],
                    op0=ALU.mult, op1=ALU.mult)
                pb = p2.next()
                R.I("tensor", "matmul", [c_rot, qn_], [pb], pb[:], c_rot[:], qn_[:], start=True, stop=True)
                R.I("gpsimd", "tensor_tensor", [qn_, cst], [t1_], out=t1_[:], in0=qn_[:], in1=cst[:, 0, :], op=ALU.mult)
                R.I("vector", "tensor_tensor", [pb, cst], [t2_], out=t2_[:], in0=pb[:], in1=cst[:, 1, :], op=ALU.mult)
                R.I("vector", "tensor_tensor", [t1_, t2_], [qf_], out=qf_[:], in0=t1_[:], in1=t2_[:], op=ALU.add)
                if j < 8:
                    dst = Qs.t[2 * j:2 * j + 2, :, it * 512:(it + 1) * 512].rearrange("h d t -> (h d) t")
                    R.D("gpsimd", [qf_], [Qs], out=dst, in_=qf_[:])
                else:
                    dst = Ks.t[2 * (j - 8):2 * (j - 8) + 2, :, it * 512:(it + 1) * 512].rearrange("h d t -> (h d) t")
                    R.D("gpsimd", [qf_], [Ks], out=dst, in_=qf_[:])
            for blk in range(4):
                ps = pm.next()
                for c in range(16):
                    R.I("tensor", "matmul", [wv, hTt], [ps], ps[:, 0:256], hTt[:, c, blk * 128:(blk + 1) * 128], wv[:, c, :], start=(c == 0), stop=(c == 15))
                v_ = vb.next()
                R.I("scalar", "activation", [ps], [v_], out=v_[:], in_=ps[:, 0:256], func=AF.Copy)
                r0 = it * 512 + blk * 128
                R.D("gpsimd", [v_], [Vs], out=Vs.t[r0:r0 + 128, :], in_=v_[:])
        R.flush()

    UT = R.dram("UT", [3072, 2, TOK], BF16)
    if "noA2" not in cfg.debug:
      with ExitStack() as st:
        c_n1w = R.sb(st, "c_n1w", [128, D], F32)
        c_idf = R.sb(st, "c_idf", [128, 128], F32)
        c_idb = R.sb(st, "c_idb", [128, 128], BF16)
        c_eps = R.sb(st, "c_eps", [128, 1], F32)
        whx = R.sb(st, "whx", [128, 16, 3072], BF16)
        R.D("sync", [n1w], [c_n1w], out=c_n1w[:], in_=n1w[:])
        R.D("sync", [ident], [c_idf], out=c_idf[:], in_=ident[:])
        R.I("vector", "tensor_copy", [c_idf], [c_idb], out=c_idb[:], in_=c_idf[:])
        R.I("vector", "memset", [], [c_eps], c_eps[:], EPS)
        for c0 in range(0, 16, 2):
            for n0 in range(0, 3072, 1536):
                R.D("gpsimd", [w_in], [whx], out=whx[:, c0:c0 + 2, n0:n0 + 1536], in_=w_in_v[:, c0:c0 + 2, 1536 + n0:1536 + n0 + 1536])
        lnt = mk_norm_T(st, c_n1w, c_idb, c_eps)
        hT = Rot([R.sb(st, "hT%d" % i, [128, 16, 512], BF16) for i in range(2)])
        pm = Rot([R.ps(st, "pm%d" % i, [128, 512], F32) for i in range(4)])
        uo = Rot([R.sb(st, "uo%d" % i, [128, 512], BF16) for i in range(4)])
        ev = 0
        for chunk, xsrc in ((0, x_own), (1, x_oth)):
            for it in range(NT):
                hTt = hT.next()
                for blk in range(4):
                    lnt(xsrc, it * 512 + blk * 128, hTt, blk)
                for j in range(24):
                    ps = pm.next()
                    for c in range(16):
                        R.I("tensor", "matmul", [whx, hTt], [ps], ps[:], whx[:, c, j * 128:(j + 1) * 128], hTt[:, c, :], start=(c == 0), stop=(c == 15))
                    u_ = uo.next()
                    if ev % 2 == 0:
                        R.I("scalar", "activation", [ps], [u_], out=u_[:], in_=ps[:], func=AF.Copy)
                    else:
                        R.I("vector", "tensor_copy", [ps], [u_], out=u_[:], in_=ps[:])
                    ev += 1
                    R.D("gpsimd", [u_], [UT], out=UT.t[j * 128:(j + 1) * 128, chunk, it * 512:(it + 1) * 512], in_=u_[:])
        R.flush()

    if "qkv" in cfg.debug:
        with ExitStack() as st:
            dq = R.dram("dbg_q", [16, 64, TOKX], BF16, kind="ExternalOutput")
            dk = R.dram("dbg_k", [4, 64, TOKX], BF16, kind="ExternalOutput")
            dv = R.dram("dbg_v", [TOKX, 256], BF16, kind="ExternalOutput")
            tq = R.sb(st, "tq", [64, 16, TOKX], BF16)
            tk = R.sb(st, "tk", [64, 4, TOKX], BF16)
            tv = R.sb(st, "tv", [128, TOKX // 128, 256], BF16)
            R.D("sync", [Qs], [tq], out=tq[:], in_=Qs.t.rearrange("h d t -> d h t"))
            R.D("sync", [tq], [dq], out=dq.t.rearrange("h d t -> d h t"), in_=tq[:])
            R.D("sync", [Ks], [tk], out=tk[:], in_=Ks.t.rearrange("h d t -> d h t"))
            R.D("sync", [tk], [dk], out=dk.t.rearrange("h d t -> d h t"), in_=tk[:])
            R.D("sync", [Vs], [tv], out=tv[:], in_=Vs.t.rearrange("(b p) c -> p b c", p=128))
            R.D("sync", [tv], [dv], out=dv.t.rearrange("(b p) c -> p b c", p=128), in_=tv[:])
            R.flush()

    if "noH" not in cfg.debug:
      with ExitStack() as st:
        c_cw = R.sb(st, "c_cw", [128, 24, 4], F32)
        c_fl = R.sb(st, "c_fl", [128, 4], F32)
        R.D("sync", [cwt], [c_cw], out=c_cw[:], in_=cwt[:])
        R.D("sync", [eflag], [c_fl], out=c_fl[:], in_=eflag[:])
        ext = Rot([R.sb(st, "ext%d" % i, [128, TOK + 2], BF16) for i in range(2)])
        edg = Rot([R.sb(st, "edg%d" % i, [128, 2], BF16) for i in range(2)])
        PW = min(2048, TOK)
        acc = Rot([R.sb(st, "acc%d" % i, [128, PW], F32) for i in range(2)])
        ucb = Rot([R.sb(st, "ucb%d" % i, [128, TOK], BF16) for i in range(2)])
        for j in range(24):
            for chunk in range(2):
                e_ = ext.next()
                d_ = edg.next()
                R.D("sync", [UT], [e_], out=e_[:, 1:TOK + 1], in_=UT.t[j * 128:(j + 1) * 128, chunk, :])
                R.D("sync", [UT], [d_], out=d_[:, 0:1], in_=UT.t[j * 128:(j + 1) * 128, 1 - chunk, TOK - 1:TOK], allow_slow_non_contiguous=True)
                R.D("sync", [UT], [d_], out=d_[:, 1:2], in_=UT.t[j * 128:(j + 1) * 128, 1 - chunk, 0:1], allow_slow_non_contiguous=True)
                R.I("vector", "tensor_scalar", [d_, c_fl], [e_], out=e_[:, 0:1], in0=d_[:, 0:1], scalar1=c_fl[:, 2 * chunk:2 * chunk + 1], scalar2=None, op0=ALU.mult)
                R.I("vector", "tensor_scalar", [d_, c_fl], [e_], out=e_[:, TOK + 1:TOK + 2], in0=d_[:, 1:2], scalar1=c_fl[:, 2 * chunk + 1:2 * chunk + 2], scalar2=None, op0=ALU.mult)
                o_ = ucb.next()
                for p0 in range(0, TOK, PW):
                    a_ = acc.next()
                    R.I("scalar", "activation", [e_, c_cw], [a_], out=a_[:], in_=e_[:, p0:p0 + PW], func=AF.Identity, bias=c_cw[:, j, 3:4], scale=c_cw[:, j, 0:1])
                    R.I("vector", "scalar_tensor_tensor", [e_, c_cw, a_], [a_], out=a_[:], in0=e_[:, p0 + 1:p0 + 1 + PW], scalar=c_cw[:, j, 1:2], in1=a_[:], op0=ALU.mult, op1=ALU.add)
                    R.I("vector", "scalar_tensor_tensor", [e_, c_cw, a_], [o_], out=o_[:, p0:p0 + PW], in0=e_[:, p0 + 2:p0 + 2 + PW], scalar=c_cw[:, j, 2:3], in1=a_[:], op0=ALU.mult, op1=ALU.add)
                R.D("gpsimd", [o_], [UC], out=UC.t[j * 128:(j + 1) * 128, chunk, :], in_=o_[:])
        R.flush()

    TWO_PI = 2.0 * math.pi
    KOFF = 16.0
    if "noH" not in cfg.debug:
      with ExitStack() as st:
        c_w1 = R.sb(st, "c_w1", [33, 64], F32)
        c_w2 = R.sb(st, "c_w2", [64, 64], F32)
        c_w3 = R.sb(st, "c_w3", [64, 64], F32)
        c_w4 = R.sb(st, "c_w4", [64, 4096], F32)
        c_w4s = R.sb(st, "c_w4s", [64, 6, 2048], BF16)
        c_fv = R.sb(st, "c_fv", [64, 4], F32)
        c_sc = R.sb(st, "c_sc", [64, 8], F32)
        c_ds = R.sb(st, "c_ds", [64, 12], F32)
        c_dl = R.sb(st, "c_dl", [128, 8], F32)
        c_npi = R.sb(st, "c_npi", [64, 1], F32)
        for dst, src in ((c_w1, f_w1), (c_w2, f_w2), (c_w3, f_w3), (c_w4, f_w4), (c_fv, f_vec), (c_ds, f_dsel), (c_dl, f_delta)):
            R.D("sync", [src], [dst], out=dst[:], in_=src[:])
        R.I("vector", "memset", [], [c_npi], c_npi[:], -math.pi)
        R.I("vector", "tensor_scalar", [c_dl], [c_dl], out=c_dl[:], in0=c_dl[:], scalar1=-1.0, scalar2=None, op0=ALU.mult)
        R.I("vector", "tensor_scalar", [c_fv], [c_sc], out=c_sc[:, 0:1], in0=c_fv[:, 0:1], scalar1=1.0 / TWO_PI, scalar2=None, op0=ALU.mult)
        for l in range(1, 4):
            R.I("vector", "tensor_scalar", [c_fv, c_sc], [c_sc], out=c_sc[:, l:l + 1], in0=c_fv[:, l:l + 1], scalar1=c_sc[:, 0:1], scalar2=KOFF + 0.5, op0=ALU.mult, op1=ALU.add)
        for fh in range(6):
            R.I("vector", "tensor_scalar", [c_w4, c_ds], [c_w4s], out=c_w4s[:, fh, :], in0=c_w4[:, 0:2048], scalar1=c_ds[:, 2 * fh:2 * fh + 1], scalar2=None, op0=ALU.mult)
            R.I("vector", "scalar_tensor_tensor", [c_w4, c_ds, c_w4s], [c_w4s], out=c_w4s[:, fh, :], in0=c_w4[:, 2048:4096], scalar=c_ds[:, 2 * fh + 1:2 * fh + 2],
                in1=c_w4s[:, fh, :], op0=ALU.mult, op1=ALU.add)
        NPT = N // 512
        NF = 3 * NPT
        nacc = R.sb(st, "nacc", [128, 2, 8, NF + 3], F32)
        c_tfl = R.sb(st, "c_tfl", [128, NF + 3], F32)
        R.D("sync", [f_cnt], [c_tfl], out=c_tfl[:], in_=f_cnt[:])
        R.I("vector", "memset", [], [nacc], nacc[:], 0.0)
        zt = Rot([R.sb(st, "zt%d" % i, [33, 512], F32) for i in range(2)])
        tb = Rot([R.sb(st, "ttb%d" % i, [128, 512], F32) for i in range(3)])
        uu = Rot([R.sb(st, "uu%d" % i, [64, 512], F32) for i in range(2)])
        ki = Rot([R.sb(st, "ki%d" % i, [64, 512], I32) for i in range(2)])
        kf = Rot([R.sb(st, "kf%d" % i, [64, 512], F32) for i in range(2)])
        hh = Rot([R.sb(st, "hh%d" % i, [64, 512], F32) for i in range(4)])
        h3b = Rot([R.sb(st, "h3b%d" % i, [64, 512], BF16) for i in range(3)])
        dec = Rot([R.sb(st, "dec%d" % i, [128, 512], F32) for i in range(3)])
        tbf = Rot([R.sb(st, "tbf%d" % i, [128, 512], BF16) for i in range(6)])
        ab = Rot([R.sb(st, "ab%d" % i, [128, 512], BF16) for i in range(2)])
        ph = Rot([R.ps(st, "ph%d" % i, [64, 512], F32) for i in range(2)])
        ptp = Rot([R.ps(st, "ptp%d" % i, [128, 512], F32) for i in range(4)])

        def sin_layer(src_ps, lcol, dst):
            u_, ki_, kf_ = uu.next(), ki.next(), kf.next()
            R.I("vector", "tensor_scalar", [src_ps, c_sc], [u_], out=u_[:], in0=src_ps[:], scalar1=c_sc[:, 0:1], scalar2=c_sc[:, lcol:lcol + 1], op0=ALU.mult, op1=ALU.add)
            yield
            R.I("vector", "tensor_copy", [u_], [ki_], out=ki_[:], in_=u_[:])
            yield
            R.I("vector", "tensor_tensor", [u_, ki_], [kf_], out=kf_[:], in0=u_[:], in1=ki_[:], op=ALU.subtract)
            yield
            R.I("vector", "scalar_tensor_tensor", [kf_], [u_], out=u_[:], in0=kf_[:], scalar=0.0, in1=kf_[:], op0=ALU.is_lt, op1=ALU.add)
            R.I("scalar", "activation", [u_, c_npi], [dst], out=dst[:], in_=u_[:], func=AF.Sin, bias=c_npi[:, 0:1], scale=TWO_PI)
            yield

        def mlp_chain(f, pt_, res):
            p0 = pt_ * 512
            z_ = zt.next()
            t_ = tb.next()
            R.D("sync", [f_zt], [z_], out=z_[:], in_=f_zt.t[f, :, p0:p0 + 512])
            R.D("sync", [f_tt], [t_], out=t_[:], in_=f_tt.t[f:f + 1, p0:p0 + 512].partition_broadcast(128))
            p_ = ph.next()
            R.I("tensor", "matmul", [c_w1, z_], [p_], p_[:], c_w1[:], z_[:], start=True, stop=True)
            h1 = hh.next()
            yield from sin_layer(p_, 1, h1)
            p_ = ph.next()
            R.I("tensor", "matmul", [c_w2, h1], [p_], p_[:], c_w2[:], h1[:], start=True, stop=True)
            h2_ = hh.next()
            yield from sin_layer(p_, 2, h2_)
            p_ = ph.next()
            R.I("tensor", "matmul", [c_w3, h2_], [p_], p_[:], c_w3[:], h2_[:], start=True, stop=True)
            h3_ = h3b.next()
            yield from sin_layer(p_, 3, h3_)
            res.append((t_, h3_))

        work = [(f, pt_) for f in range(3) for pt_ in range(NPT)]
        res = []
        for _ in mlp_chain(work[0][0], work[0][1], res):
            pass
        for wi, (f, pt_) in enumerate(work):
            p0 = pt_ * 512
            half = 0 if p0 < TOK else 1
            fh = f * 2 + half
            t_, h3_ = res[wi]
            nxt = mlp_chain(work[wi + 1][0], work[wi + 1][1], res) if wi + 1 < len(work) else iter(())
            for ct in range(8):
                d_ = dec.next()
                R.I("scalar", "activation", [t_, c_dl], [d_], out=d_[:], in_=t_[:], func=AF.Exp, scale=c_dl[:, ct:ct + 1])
                for o in range(2):
                    pp_ = ptp.next()
                    R.I("tensor", "matmul", [c_w4s, h3_], [pp_], pp_[:], c_w4s[:, fh, o * 1024 + ct * 128:o * 1024 + (ct + 1) * 128], h3_[:], start=True, stop=True)
                    tb_, ab_ = tbf.next(), ab.next()
                    R.I("vector", "tensor_tensor", [pp_, d_], [tb_], out=tb_[:], in0=pp_[:], in1=d_[:], op=ALU.mult)
                    R.D("gpsimd", [tb_], [TAPS], out=TAPS.t[f, o, ct * 128:(ct + 1) * 128, p0:p0 + 512], in_=tb_[:])
                    R.I("scalar", "activation", [tb_], [ab_, nacc], out=ab_[:], in_=tb_[:], func=AF.Abs, accum_out=nacc[:, o, ct, f * NPT + pt_:f * NPT + pt_ + 1])
                    if pt_ == 0:
                        R.I("scalar", "activation", [tb_], [nacc], out=nacc[:, o, ct, NF + f:NF + f + 1], in_=tb_[:, 0:1], func=AF.Abs)
                    next(nxt, None)
            for _ in nxt:
                pass
        nrm = R.sb(st, "nrm", [128, 2, 8], F32)
        njk = R.sb(st, "njk", [128, NF + 3], F32)
        for o in range(2):
            for ct in range(8):
                R.I("vector", "scalar_tensor_tensor", [nacc, c_tfl], [njk, nrm], out=njk[:], in0=nacc[:, o, ct, :], scalar=1.0, in1=c_tfl[:], op0=ALU.mult, op1=ALU.mult,
                    accum_out=nrm[:, o, ct:ct + 1])
        R.I("vector", "reciprocal", [nrm], [nrm], out=nrm[:], in_=nrm[:])
        R.D("sync", [nrm], [RN], out=RN.t, in_=nrm[:].rearrange("p o c -> p (o c)"))
        R.flush()

    NGRP = 1024 // CG
    KP = 13 if KL % 13 == 0 else 3
    assert KL % KP == 0

    def fwd_phase(jobs):
        with ExitStack() as st:
            f1m = R.sb(st, "f1m", [128, 2 * KL], BF16)
            f2m = R.sb(st, "f2m", [128, KL, 3, 128], BF16)
            R.D("gpsimd", [m_f1], [f1m], out=f1m[0:NHI, :], in_=m_f1[:])
            for k0 in range(0, KL, 8):
                k1 = min(KL, k0 + 8)
                R.D("gpsimd", [m_f2], [f2m], out=f2m[:, k0:k1, 0:2, :], in_=m_f2[:, k0:k1, :, :])
            R.I("vector", "tensor_scalar", [f2m], [f2m], out=f2m[:, :, 2, :], in0=f2m[:, :, 1, :], scalar1=-1.0, scalar2=None, op0=ALU.mult)
            xin = Rot([R.sb(st, "xin%d" % i, [128, CG, 128], BF16) for i in range(2)])
            Ar = Rot([R.sb(st, "Afft%d" % i, [128, KL, 2, CG], BF16) for i in range(2)])
            X = Rot([R.sb(st, "Xfft%d" % i, [128, KL, 2, CG], BF16) for i in range(2)])
            Xs = R.sb(st, "Xsw", [128, KL, 2, CG], BF16)
            pF1 = Rot([R.ps(st, "pF1_%d" % i, [128, 4, 256], F32) for i in range(2)])
            pX = Rot([R.ps(st, "pX%d" % i, [128, 8, 2, CG], F32) for i in range(2)])
            ev = 0
            for (src_fn, src_obj, KIN, dst_obj, dst_fn, sw_fn) in jobs:
                for g in range(NGRP):
                    xi = xin.next()
                    A = Ar.next()
                    for cq in range(0, CG, 16):
                        R.D("sync", [src_obj], [xi], out=xi[0:KIN, cq:cq + 16, :], in_=src_fn(g)[cq:cq + 16, :].rearrange("c (h l) -> h c l", l=128))
                    for c in range(0, CG, 4):
                        p = pF1.next()
                        for i in range(4):
                            R.I("tensor", "matmul", [xi, f1m], [p], p[:, i, 0:2 * KL], xi[0:KIN, c + i, :], f1m[0:KIN, :], start=True, stop=True)
                        src = p[:, :, 0:2 * KL].rearrange("p c (r k) -> p k r c", r=2)
                        if ev % 2 == 0:
                            R.I("scalar", "activation", [p], [A], out=A[:, :, :, c:c + 4], in_=src, func=AF.Copy)
                        else:
                            R.I("vector", "tensor_copy", [p], [A], out=A[:, :, :, c:c + 4], in_=src)
                        ev += 1
                    Xt = X.next()
                    k0 = 0
                    while k0 < KL:
                        n_ = min(8, KL - k0)
                        p = pX.next()
                        for i in range(n_):
                            kk = k0 + i
                            R.I("tensor", "matmul", [f2m, A], [p], p[:, i, 0, :], f2m[:, kk, 0, :], A[:, kk, 0, :], start=True, stop=False)
                            R.I("tensor", "matmul", [f2m, A], [p], p[:, i, 0, :], f2m[:, kk, 2, :], A[:, kk, 1, :], start=False, stop=True)
                            R.I("tensor", "matmul", [f2m, A], [p], p[:, i, 1, :], f2m[:, kk, 1, :], A[:, kk, 0, :], start=True, stop=False)
                            R.I("tensor", "matmul", [f2m, A], [p], p[:, i, 1, :], f2m[:, kk, 0, :], A[:, kk, 1, :], start=False, stop=True)
                        if ev % 2 == 0:
                            R.I("scalar", "activation", [p], [Xt], out=Xt[:, k0:k0 + n_, :, :], in_=p[:, 0:n_, :, :], func=AF.Copy)
                        else:
                            R.I("vector", "tensor_copy", [p], [Xt], out=Xt[:, k0:k0 + n_, :, :], in_=p[:, 0:n_, :, :])
                        ev += 1
                        k0 += n_
                    R.D("gpsimd", [Xt], [dst_obj], out=dst_fn(g), in_=Xt[:].rearrange("p k r c -> p (k r c)"))
                    if sw_fn is not None:
                        R.I("vector", "tensor_copy", [Xt], [Xs], out=Xs[:, :, 0, :], in_=Xt[:, :, 1, :])
                        R.I("vector", "tensor_copy", [Xt], [Xs], out=Xs[:, :, 1, :], in_=Xt[:, :, 0, :])
                        R.D("gpsimd", [Xs], [GSW], out=sw_fn(g), in_=Xs[:].rearrange("p k r c -> p (k r c)"))
            R.flush()

    def inv_phase(jobs):
        with ExitStack() as st:
            i1m = R.sb(st, "i1m", [128, 2, 256], BF16)
            i2m = R.sb(st, "i2m", [128, 128, 2, NB], BF16)
            R.D("gpsimd", [m_i1], [i1m], out=i1m[:], in_=m_i1[:])
            LSTEP = max(1, 2048 // (2 * NB))
            for l0 in range(0, 128, LSTEP):
                R.D("gpsimd", [m_i2], [i2m], out=i2m[0:KL, l0:l0 + LSTEP, :, :], in_=m_i2[:, l0:l0 + LSTEP, :, :])
            idp = R.sb(st, "idp", [128, 128], BF16)
            idn = R.sb(st, "idn", [128, 128], BF16)
            R.D("gpsimd", [ident], [idp], out=idp[:], in_=ident[:])
            R.I("vector", "tensor_scalar", [idp], [idn], out=idn[:], in0=idp[:], scalar1=-1.0, scalar2=None, op0=ALU.mult)
            c_rn = R.sb(st, "c_rn", [64, 2, 8, 2], F32)
            c_nb = R.sb(st, "c_nb", [64, 2, 8, 2], F32)
            c_hb = R.sb(st, "c_hb", [64, 2, 16], F32)
            R.D("sync", [RN], [c_rn], out=c_rn[:], in_=RN.t.rearrange("(par p) (o c) -> p o c par", par=2, o=2), allow_slow_non_contiguous=True)
            R.D("sync", [hyb], [c_hb], out=c_hb[:], in_=hyb[:])
            R.I("vector", "reciprocal", [c_rn], [c_nb], out=c_nb[:], in_=c_rn[:])
            R.I("vector", "tensor_tensor", [c_nb, c_hb], [c_nb], out=c_nb[:].rearrange("p o c par -> p o (c par)"), in0=c_nb[:].rearrange("p o c par -> p o (c par)"), in1=c_hb[:], op=ALU.mult)
            pc = [Rot([R.sb(st, "pc%d_%d" % (j, i), [128, KP, 2, CG], BF16) for i in range(2)]) for j in range(6)]
            pr = [R.sb(st, "pr%d" % j, [128, KP, 2, CG], BF16) for j in range(4)]
            Y = R.sb(st, "Yfft", [128, KL, 2, CG], BF16)
            Cb = R.sb(st, "Cb", [128, 128, 2, CG], BF16)
            gt = R.sb(st, "gt", [CG, TOK], BF16)
            zt_ = R.sb(st, "zt_", [CG, TOK], BF16)
            ot = R.sb(st, "ot_", [CG, TOK], BF16)
            pYr = R.ps(st, "pYr", [128, 8, CG], F32)
            pYi = R.ps(st, "pYi", [128, 8, CG], F32)
            pC = Rot([R.ps(st, "pC%d" % i, [128, 4, 256], F32) for i in range(2)])
            pY = Rot([R.ps(st, "pY%d" % i, [CG, 8, NB], F32) for i in range(2)])
            ev = 0
            for job in jobs:
                o = job["order"]
                for g in range(NGRP):
                    rn_ap = c_rn[:, o, g // 2, (g % 2):(g % 2) + 1]
                    nb_ap = c_nb[:, o, g // 2, (g % 2):(g % 2) + 1]
                    R.D("sync", [UC], [gt], out=gt[:], in_=job["gate_fn"](g))
                    R.D("sync", [job["z_obj"]], [zt_], out=zt_[:], in_=job["z_fn"](g))
                    R.I("scalar", "activation", [gt, c_rn], [gt], out=gt[:], in_=gt[:], func=AF.Copy, scale=rn_ap)
                    R.I("vector", "tensor_scalar", [zt_, c_nb], [zt_], out=zt_[:], in0=zt_[:], scalar1=nb_ap, scalar2=None, op0=ALU.mult)
                    (Xa, Xaf, gsa), (Xb, Xbf, gsb) = job["terms"]
                    for k0 in range(0, KL, KP):
                        ks = slice(k0 * 2 * CG, (k0 + KP) * 2 * CG)
                        xa, ga, gas, xb_, gb, gbs = [pc[j].next() for j in range(6)]
                        for (t_, obj, ap_) in ((xa, Xa, Xaf(g)), (ga, GSP, GSP.t[gsa, g]), (gas, GSW, GSW.t[gsa, g]),
                                               (xb_, Xb, Xbf(g)), (gb, GSP, GSP.t[gsb, g]), (gbs, GSW, GSW.t[gsb, g])):
                            R.D("sync", [obj], [t_], out=t_[:].rearrange("p k r c -> p (k r c)"), in_=ap_[:, ks])
                        R.I("vector", "tensor_tensor", [xa, ga], [pr[0]], out=pr[0][:], in0=xa[:], in1=ga[:], op=ALU.mult)
                        R.I("vector", "tensor_tensor", [xa, gas], [pr[1]], out=pr[1][:], in0=xa[:], in1=gas[:], op=ALU.mult)
                        R.I("vector", "tensor_tensor", [xb_, gb], [pr[2]], out=pr[2][:], in0=xb_[:], in1=gb[:], op=ALU.mult)
                        R.I("vector", "tensor_tensor", [xb_, gbs], [pr[3]], out=pr[3][:], in0=xb_[:], in1=gbs[:], op=ALU.mult)
                        for j0 in range(0, KP, 8):
                            j1 = min(KP, j0 + 8)
                            nj = j1 - j0
                            R.I("tensor", "matmul", [idp, pr[0]], [pYr], pYr[:, 0:nj, :], idp[:], pr[0][:, j0:j1, 0, :], start=True, stop=False)
                            R.I("tensor", "matmul", [idn, pr[0]], [pYr], pYr[:, 0:nj, :], idn[:], pr[0][:, j0:j1, 1, :], start=False, stop=False)
                            R.I("tensor", "matmul", [idp, pr[2]], [pYr], pYr[:, 0:nj, :], idp[:], pr[2][:, j0:j1, 0, :], start=False, stop=False)
                            R.I("tensor", "matmul", [idn, pr[2]], [pYr], pYr[:, 0:nj, :], idn[:], pr[2][:, j0:j1, 1, :], start=False, stop=True)
                            R.I("tensor", "matmul", [idp, pr[1]], [pYi], pYi[:, 0:nj, :], idp[:], pr[1][:, j0:j1, 0, :], start=True, stop=False)
                            R.I("tensor", "matmul", [idp, pr[1]], [pYi], pYi[:, 0:nj, :], idp[:], pr[1][:, j0:j1, 1, :], start=False, stop=False)
                            R.I("tensor", "matmul", [idp, pr[3]], [pYi], pYi[:, 0:nj, :], idp[:], pr[3][:, j0:j1, 0, :], start=False, stop=False)
                            R.I("tensor", "matmul", [idp, pr[3]], [pYi], pYi[:, 0:nj, :], idp[:], pr[3][:, j0:j1, 1, :], start=False, stop=True)
                            R.I("scalar", "activation", [pYr], [Y], out=Y[:, k0 + j0:k0 + j1, 0, :], in_=pYr[:, 0:nj, :], func=AF.Copy)
                            R.I("scalar", "activation", [pYi], [Y], out=Y[:, k0 + j0:k0 + j1, 1, :], in_=pYi[:, 0:nj, :], func=AF.Copy)
                    for c in range(0, CG, 4):
                        p = pC.next()
                        for i in range(4):
                            R.I("tensor", "matmul", [Y, i1m], [p], p[0:KL, i, :], Y[:, :, 0, c + i], i1m[:, 0, :], start=True, stop=False)
                            R.I("tensor", "matmul", [Y, i1m], [p], p[0:KL, i, :], Y[:, :, 1, c + i], i1m[:, 1, :], start=False, stop=True)
                        src = p[0:KL, :, :].rearrange("p c (r l) -> p l r c", r=2)
                        if ev % 2 == 0:
                            R.I("scalar", "activation", [p], [Cb], out=Cb[0:KL, :, :, c:c + 4], in_=src, func=AF.Copy)
                        else:
                            R.I("vector", "tensor_copy", [p], [Cb], out=Cb[0:KL, :, :, c:c + 4], in_=src)
                        ev += 1
                    for l0 in range(0, 128, 8):
                        p = pY.next()
                        zqv = zt_[:].rearrange("c (h l) -> c l h", l=128)[:, l0:l0 + 8, :]
                        gtv = gt[:].rearrange("c (h l) -> c l h", l=128)[:, l0:l0 + 8, :]
                        otv = ot[:].rearrange("c (h l) -> c l h", l=128)[:, l0:l0 + 8, :]
                        R.I("tensor", "matmul", [idp, zt_], [p], p[:], idp[0:CG, 0:CG], zqv, start=True, stop=False)
                        for i in range(8):
                            nl = l0 + i
                            R.I("tensor", "matmul", [Cb, i2m], [p], p[:, i, :], Cb[0:KL, nl, 0, :], i2m[0:KL, nl, 0, :], start=False, stop=False)
                            R.I("tensor", "matmul", [Cb, i2m], [p], p[:, i, :], Cb[0:KL, nl, 1, :], i2m[0:KL, nl, 1, :], start=False, stop=(i == 7))
                        R.I("vector", "tensor_tensor", [p, gt], [ot], out=otv, in0=p[:], in1=gtv, op=ALU.mult)
                    R.D("gpsimd", [ot], [job["dst_obj"]], out=job["dst_fn"](g), in_=ot[:])
            R.flush()

    if "noH" not in cfg.debug:
        GIDX = {(0, 0): 0, (1, 0): 1, (2, 0): 2, (0, 1): 3, (1, 1): 4}
        jobs = []
        for (f, o), gi in GIDX.items():
            jobs.append((lambda g, f=f, o=o: TAPS.t[f, o, g * CG:(g + 1) * CG, :], TAPS, NHI, GSP, (lambda g, gi=gi: GSP.t[gi, g]), (lambda g, gi=gi: GSW.t[gi, g])))
        for ch in range(2):
            jobs.append((lambda g, ch=ch: UC.t[2048 + g * CG:2048 + (g + 1) * CG, ch, :], UC, NB, XSP, (lambda g, ch=ch: XSP.t[ch, g]), None))
        fwd_phase(jobs)
        ijobs = []
        for ch in range(2):
            gx = GIDX[(1 if ch == 0 else 2, 0)]
            ijobs.append(dict(order=0,
                              terms=[(XSP, (lambda g, ch=ch: XSP.t[ch, g]), GIDX[(0, 0)]),
                                     (XSP, (lambda g, ch=ch: XSP.t[1 - ch, g]), gx)],
                              gate_fn=(lambda g, ch=ch: UC.t[g * CG:(g + 1) * CG, ch, :]),
                              z_obj=UC, z_fn=(lambda g, ch=ch: UC.t[2048 + g * CG:2048 + (g + 1) * CG, ch, :]),
                              dst_obj=Z1, dst_fn=(lambda g, ch=ch: Z1.t[g * CG:(g + 1) * CG, ch, :])))
        inv_phase(ijobs)
        jobs = []
        for ch in range(2):
            jobs.append((lambda g, ch=ch: Z1.t[g * CG:(g + 1) * CG, ch, :], Z1, NB, XSP, (lambda g, ch=ch: XSP.t[ch, g]), None))
        fwd_phase(jobs)
        inv_phase([dict(order=1,
                        terms=[(XSP, (lambda g: XSP.t[0, g]), GIDX[(0, 1)]),
                               (XSP, (lambda g: XSP.t[1, g]), GIDX[(1, 1)])],
                        gate_fn=(lambda g: UC.t[1024 + g * CG:1024 + (g + 1) * CG, 0, :]),
                        z_obj=Z1, z_fn=(lambda g: Z1.t[g * CG:(g + 1) * CG, 0, :]),
                        dst_obj=YH, dst_fn=(lambda g: YH.t[g * CG:(g + 1) * CG, :]))])

    if "zeroYH" in cfg.debug:
        with ExitStack() as st:
            z = R.sb(st, "zt", [128, TOK], BF16)
            R.I("vector", "memset", [], [z], z[:], 0.0)
            for c in range(8):
                R.D("sync", [z], [YH], out=YH.t[c * 128:(c + 1) * 128, :], in_=z[:])
            R.flush()
    ost = ExitStack()
    rt_oh = R.sb(ost, "rt_oh", [128, NB, 2, NE], BF16)
    rt_w = R.sb(ost, "rt_w", [128, NB, 2], F32)
    rt_e = R.sb(ost, "rt_e", [128, NB, 2], F32)
    w_out_v = w_out.t.rearrange("(c p) n -> p c n", p=128)
    w_r_v = w_r.t.rearrange("(c p) n -> p c n", p=128)
    if "noB" not in cfg.debug:
      with ExitStack() as st:
        Wa = R.sb(st, "Wa", [128, 8, D], BF16)
        Wh = R.sb(st, "Wh", [128, 8, D], BF16)
        for c0 in range(0, 8, 2):
            R.D("gpsimd", [w_out], [Wa], out=Wa[:, c0:c0 + 2, :], in_=w_out_v[:, c0:c0 + 2, :])
            R.D("gpsimd", [w_out], [Wh], out=Wh[:, c0:c0 + 2, :], in_=w_out_v[:, 8 + c0:8 + c0 + 2, :])
        c_aon = R.sb(st, "c_aon", [128, 8], F32)
        c_hyn = R.sb(st, "c_hyn", [128, 8], F32)
        c_n2w = R.sb(st, "c_n2w", [128, D], F32)
        c_wr = R.sb(st, "c_wr", [128, 16, NGE], F32)
        c_br = R.sb(st, "c_br", [128, NGE], F32)
        c_iot = R.sb(st, "c_iot", [128, NE], F32)
        c_idf = R.sb(st, "c_idf", [128, 128], F32)
        c_eps = R.sb(st, "c_eps", [128, 1], F32)
        c_msk = R.sb(st, "c_msk", [128, 4, 512], BF16)
        c_snk = R.sb(st, "c_snk", [128, 8], F32)
        c_esk = R.sb(st, "c_esk", [128, 8, 128], F32)
        c_one = R.sb(st, "c_one", [128, 64], BF16)
        c_onf = R.sb(st, "c_onf", [128, 1], F32)
        for dst, src in ((c_aon, aonw), (c_hyn, hynw), (c_n2w, n2w), (c_br, b_r), (c_iot, iota_e), (c_idf, ident), (c_snk, sink2)):
            R.D("sync", [src], [dst], out=dst[:], in_=src[:])
        R.D("sync", [w_r], [c_wr], out=c_wr[:], in_=w_r_v)
        R.D("gpsimd", [masks], [c_msk], out=c_msk[:], in_=masks.t.rearrange("m p n -> p m n"))
        R.I("vector", "memset", [], [c_eps], c_eps[:], EPS)
        R.I("vector", "memset", [], [c_one], c_one[:], 1.0)
        R.I("vector", "memset", [], [c_onf], c_onf[:], 1.0)
        R.I("scalar", "activation", [c_snk], [c_snk], out=c_snk[:], in_=c_snk[:], func=AF.Exp)
        R.I("vector", "memset", [], [c_esk], c_esk[:], 0.0)
        for i in range(8):
            R.I("vector", "tensor_scalar", [c_esk, c_snk], [c_esk], out=c_esk[:, i, :], in0=c_esk[:, i, :], scalar1=c_snk[:, i:i + 1], scalar2=None, op0=ALU.add)
        for c in range(8):
            R.I("vector", "tensor_scalar", [Wa, c_aon], [Wa], out=Wa[:, c, :], in0=Wa[:, c, :], scalar1=c_aon[:, c:c + 1], scalar2=None, op0=ALU.mult)
            R.I("gpsimd", "tensor_scalar", [Wh, c_hyn], [Wh], out=Wh[:, c, :], in0=Wh[:, c, :], scalar1=c_hyn[:, c:c + 1], scalar2=None, op0=ALU.mult)

        qT = R.sb(st, "qT", [64, 16, 512], BF16)
        kT = R.sb(st, "kT", [64, 4, 768], BF16)
        Vt = R.sb(st, "Vt", [128, 6, 256], BF16)
        yh = Rot([R.sb(st, "yh%d" % i, [128, 8, 128], BF16) for i in range(2)])
        xt = Rot([R.sb(st, "xt%d" % i, [128, D], F32) for i in range(2)])
        x1 = Rot([R.sb(st, "x1_%d" % i, [128, D], F32) for i in range(1)])
        h2f = R.sb(st, "h2f", [128, D], F32)
        h2b = Rot([R.sb(st, "h2b%d" % i, [128, D], BF16) for i in range(1)])
        junk = R.sb(st, "junk", [128, D], BF16)
        h2T = R.sb(st, "h2T", [128, 16, 128], F32)
        sqa = R.sb(st, "sqa", [128, 8, 128], F32)
        yraw = R.sb(st, "yraw", [128, 2, 128], F32)
        ya = Rot([R.sb(st, "ya%d" % i, [128, 8, 128], BF16) for i in range(2)])
        sqh = R.sb(st, "sqh", [128, 8, 128], F32)
        pt = Rot([R.sb(st, "pt%d" % i, [128, 4, 128], BF16) for i in range(4)])
        dn = R.sb(st, "dn", [128, 2, 128], F32)
        sm = Rot([R.sb(st, "sm%d" % i, [128, 128], F32) for i in range(3)])
        pS = Rot([R.ps(st, "pS%d" % i, [128, 512], F32) for i in range(3)])
        pdx = R.ps(st, "pdx", [128, 512], F32)
        pden = Obj("pden_v", pdx.t[:, 0:256].rearrange("p (a b) -> p a b", a=2))
        po = R.ps(st, "po", [128, 2, 128], F32)
        ppa = R.ps(st, "ppa", [128, 512], F32)
        ppb = R.ps(st, "ppb", [128, 512], F32)
        ptr = R.ps(st, "ptr", [128, 4, 128], F32)
        psm = Obj("psm_v", pdx.t[:, 256:320])
        junk2 = R.sb(st, "junk2", [128, NE], F32)

        def router_chain(b, s_):
            R.I("vector", "tensor_reduce", [s_], [s_], out=s_[:, 5:6], in_=s_[:, 16:16 + NG], axis=AX.X, op=ALU.max)
            ohg = s_[:, 72:72 + NG]
            R.I("vector", "tensor_scalar", [s_], [s_], out=ohg, in0=s_[:, 16:16 + NG], scalar1=s_[:, 5:6], scalar2=None, op0=ALU.is_equal)
            R.I("vector", "tensor_scalar", [s_], [s_], out=s_[:, 6:7], in0=s_[:, 5:6], scalar1=-1.0, scalar2=None, op0=ALU.mult)
            R.I("scalar", "activation", [s_], [s_], out=s_[:, 76:76 + NG], in_=s_[:, 16:16 + NG], func=AF.Exp, bias=s_[:, 6:7], scale=1.0, accum_out=s_[:, 7:8])
            yield
            sel = s_[:, 80:80 + EPG]
            R.I("vector", "tensor_scalar", [s_], [s_], out=sel, in0=s_[:, 16 + NG:16 + NG + EPG], scalar1=s_[:, 72:73], scalar2=None, op0=ALU.mult)
            for gg in range(1, NG):
                R.I("vector", "scalar_tensor_tensor", [s_], [s_], out=sel, in0=s_[:, 16 + NG + gg * EPG:16 + NG + (gg + 1) * EPG], scalar=s_[:, 72 + gg:73 + gg],
                    in1=sel, op0=ALU.mult, op1=ALU.add)
            m8 = s_[:, 64:72]
            R.I("vector", "max", [s_], [s_], out=m8, in_=sel)
            yield
            R.I("vector", "tensor_scalar", [s_], [s_], out=s_[:, 6:7], in0=s_[:, 64:65], scalar1=-1.0, scalar2=None, op0=ALU.mult)
            R.I("scalar", "activation", [s_], [s_], out=s_[:, 5:6], in_=s_[:, 65:66], func=AF.Exp, bias=s_[:, 6:7], scale=1.0)
            R.I("vector", "tensor_scalar", [s_], [s_], out=s_[:, 6:7], in0=s_[:, 5:6], scalar1=1.0, scalar2=s_[:, 7:8], op0=ALU.add, op1=ALU.mult)
            R.I("vector", "reciprocal", [s_], [rt_w], out=rt_w[:, b, 0:1], in_=s_[:, 6:7])
            R.I("vector", "tensor_tensor", [s_, rt_w], [rt_w], out=rt_w[:, b, 1:2], in0=s_[:, 5:6], in1=rt_w[:, b, 0:1], op=ALU.mult)
            yield
            for k in range(2):
                ohl = s_[:, 96 + k * 8:96 + k * 8 + EPG]
                R.I("vector", "tensor_scalar", [s_], [s_], out=ohl, in0=sel, scalar1=s_[:, 64 + k:65 + k], scalar2=None, op0=ALU.is_equal)
                for gg in range(NG):
                    R.I("vector", "tensor_scalar", [s_], [rt_oh], out=rt_oh[:, b, k, gg * EPG:(gg + 1) * EPG], in0=ohl, scalar1=s_[:, 72 + gg:73 + gg],
                        scalar2=None, op0=ALU.mult)
                R.I("vector", "scalar_tensor_tensor", [rt_oh, c_iot], [junk2, rt_e], out=junk2[:, 0:NE], in0=rt_oh[:, b, k, :], scalar=1.0, in1=c_iot[:],
                    op0=ALU.mult, op1=ALU.mult, accum_out=rt_e[:, b, k:k + 1])
                yield

        pend = iter(())
        mq = [0]

        for it in range(NT):
            t0 = it * 512
            R.D("sync", [Qs], [qT], out=qT[:], in_=Qs.t[:, :, t0:t0 + 512].rearrange("h d t -> d h t"))
            R.D("sync", [Ks], [kT], out=kT[:, :, 128:640], in_=Ks.t[:, :, t0:t0 + 512].rearrange("h d t -> d h t"))
            pv0 = TOK if it == 0 else t0 - 128
            nx0 = TOK + 128 if it == NT - 1 else t0 + 512
            R.D("sync", [Ks], [kT], out=kT[:, :, 0:128], in_=Ks.t[:, :, pv0:pv0 + 128].rearrange("h d t -> d h t"))
            R.D("sync", [Ks], [kT], out=kT[:, :, 640:768], in_=Ks.t[:, :, nx0:nx0 + 128].rearrange("h d t -> d h t"))
            R.D("sync", [Vs], [Vt], out=Vt[:, 1:5, :], in_=Vs.t[t0:t0 + 512, :].rearrange("(b p) c -> p b c", p=128))
            R.D("sync", [Vs], [Vt], out=Vt[:, 0, :], in_=Vs.t[pv0:pv0 + 128, :])
            R.D("sync", [Vs], [Vt], out=Vt[:, 5, :], in_=Vs.t[nx0:nx0 + 128, :])
            for qb in range(4):
                b = it * 4 + qb
                tk0 = t0 + qb * 128
                ya_ = ya.next()
                items = [(g, kbi) for g in range(4) for kbi in range(3)]
                Sq = []

                def emit_S(g, kbi):
                    S = pS.next()
                    R.I("tensor", "matmul", [kT, qT], [S], S[:], kT[:, g, (qb + kbi) * 128:(qb + kbi + 1) * 128],
                        qT[:, 4 * g:4 * g + 4, qb * 128:(qb + 1) * 128], start=True, stop=True)
                    Sq.append(S)

                emit_S(*items[0])
                emit_S(*items[1])
                for ii, (g, kbi) in enumerate(items):
                    if ii + 2 < len(items):
                        emit_S(*items[ii + 2])
                    S = Sq[ii]
                    p_ = pt.next()
                    R.I("scalar", "activation", [S], [p_], out=p_[:], in_=S[:].rearrange("p (h q) -> p h q", h=4), func=AF.Exp, scale=0.125)
                    mi = None
                    if kbi == 0:
                        mi = 2 if b == 0 else 0
                    elif kbi == 2:
                        mi = 3 if b == NB - 1 else 1
                    if mi is not None:
                        R.I("vector", "tensor_tensor", [p_, c_msk], [p_], out=p_[:], in0=p_[:], in1=c_msk[:, mi, :].rearrange("p (h q) -> p h q", h=4), op=ALU.mult)
                    for par in range(2):
                        R.I("tensor", "matmul", [c_one, p_], [pden], pden[par * 64:(par + 1) * 64, :, :], c_one[:, 0:64], p_[:, par::2, :],
                            start=(kbi == 0), stop=(kbi == 2))
                    for par in range(2):
                        R.I("tensor", "matmul", [Vt, p_], [po], po[par * 64:(par + 1) * 64, :, :], Vt[:, qb + kbi, g * 64:(g + 1) * 64], p_[:, par::2, :],
                            start=(kbi == 0), stop=(kbi == 2))
                    if kbi == 2:
                        R.I("vector", "tensor_tensor", [pden, c_esk], [dn], out=dn[:], in0=pden[:], in1=c_esk[:, 2 * g:2 * g + 2, :], op=ALU.add)
                        R.I("vector", "reciprocal", [dn], [dn], out=dn[:], in_=dn[:])
                        R.I("vector", "tensor_tensor", [po, dn], [yraw], out=yraw[:], in0=po[:], in1=dn[:], op=ALU.mult)
                        R.I("scalar", "activation", [yraw], [sqa], out=sqa[:, 2 * g:2 * g + 2, :], in_=yraw[:], func=AF.Square)
                        R.I("scalar", "activation", [yraw], [ya_], out=ya_[:, 2 * g:2 * g + 2, :], in_=yraw[:], func=AF.Copy)
                        next(pend, None)
                yh_ = yh.next()
                R.D("sync", [YH], [yh_], out=yh_[:], in_=YH.t[:, tk0:tk0 + 128].rearrange("(c p) t -> p c t", p=128))
                R.I("scalar", "activation", [yh_], [sqh], out=sqh[:], in_=yh_[:], func=AF.Square)
                for c in range(8):
                    R.I("tensor", "matmul", [sqa, c_onf], [psm], psm[:, 0:1], sqa[:, c, :], c_onf[:, 0:1], start=(c == 0), stop=(c == 7))
                for c in range(8):
                    R.I("tensor", "matmul", [sqh, c_onf], [psm], psm[:, 1:2], sqh[:, c, :], c_onf[:, 0:1], start=(c == 0), stop=(c == 7))
                s_ = sm.next()
                R.I("scalar", "activation", [psm, c_eps], [s_], out=s_[:, 0:2], in_=psm[:, 0:2], func=AF.Sqrt, bias=c_eps[:, 0:1], scale=1.0 / 1024)
                R.I("vector", "reciprocal", [s_], [s_], out=s_[:, 0:2], in_=s_[:, 0:2])
                xt_ = xt.next()
                x1_ = x1.next()
                R.D("sync", [x_own], [xt_], out=xt_[:], in_=x_own[tk0:tk0 + 128, :])
                for ct in range(4):
                    cs_ = slice(ct * 512, (ct + 1) * 512)
                    for c in range(8):
                        R.I("tensor", "matmul", [ya_, Wa], [ppa], ppa[:], ya_[:, c, :], Wa[:, c, cs_], start=(c == 0), stop=(c == 7))
                    for c in range(8):
                        R.I("tensor", "matmul", [yh_, Wh], [ppb], ppb[:], yh_[:, c, :], Wh[:, c, cs_], start=(c == 0), stop=(c == 7))
                    R.I("vector", "scalar_tensor_tensor", [ppa, s_, xt_], [x1_], out=x1_[:, cs_], in0=ppa[:], scalar=s_[:, 0:1], in1=xt_[:, cs_], op0=ALU.mult, op1=ALU.add)
                    R.I("vector", "scalar_tensor_tensor", [ppb, s_, x1_], [x1_], out=x1_[:, cs_], in0=ppb[:], scalar=s_[:, 1:2], in1=x1_[:, cs_], op0=ALU.mult, op1=ALU.add)
                    next(pend, None)
                R.D("gpsimd", [x1_], [X1], out=X1.t[tk0:tk0 + 128, :], in_=x1_[:])
                R.I("scalar", "activation", [x1_], [junk, s_], out=junk[:], in_=x1_[:], func=AF.Square, accum_out=s_[:, 2:3])
                R.I("scalar", "activation", [s_, c_eps], [s_], out=s_[:, 3:4], in_=s_[:, 2:3], func=AF.Sqrt, bias=c_eps[:, 0:1], scale=1.0 / D)
                R.I("vector", "reciprocal", [s_], [s_], out=s_[:, 4:5], in_=s_[:, 3:4])
                R.I("vector", "scalar_tensor_tensor", [x1_, s_, c_n2w], [h2f], out=h2f[:], in0=x1_[:], scalar=s_[:, 4:5], in1=c_n2w[:], op0=ALU.mult, op1=ALU.mult)
                hb_ = h2b.next()
                R.I("scalar", "activation", [h2f], [hb_], out=hb_[:], in_=h2f[:], func=AF.Copy)
                R.D("gpsimd", [hb_], [H2], out=H2.t[tk0:tk0 + 128, :], in_=hb_[:])
                for c4 in range(4):
                    for c in range(4):
                        cc = c4 * 4 + c
                        R.I("tensor", "transpose", [h2f, c_idf], [ptr], out=ptr[:, c, :], in_=h2f[:, cc * 128:(cc + 1) * 128], identity=c_idf[:])
                    if c4 % 2 == 0:
                        R.I("scalar", "activation", [ptr], [h2T], out=h2T[:, c4 * 4:c4 * 4 + 4, :], in_=ptr[:], func=AF.Copy)
                    else:
                        R.I("vector", "tensor_copy", [ptr], [h2T], out=h2T[:, c4 * 4:c4 * 4 + 4, :], in_=ptr[:])
                for c in range(16):
                    R.I("tensor", "matmul", [h2T, c_wr], [psm], psm[:, 8:8 + NGE], h2T[:, c, :], c_wr[:, c, :], start=(c == 0), stop=(c == 15))
                lg = s_[:, 16:16 + NGE]
                R.I("vector", "tensor_tensor", [psm, c_br], [s_], out=lg, in0=psm[:, 8:8 + NGE], in1=c_br[:], op=ALU.add)
                for _ in pend:
                    pass
                pend = router_chain(b, s_)
        for _ in pend:
            pass
        R.flush()

    slots_i = R.sb(ost, "slots_i", [128, NB, 2], I32)
    if "noC" not in cfg.debug:
      with ExitStack() as st:
        c_tri = R.sb(st, "c_tri", [128, 128], F32)
        c_trb = R.sb(st, "c_trb", [128, 128], BF16)
        c_onb = R.sb(st, "c_onb", [128, 128], BF16)
        c_tok = R.sb(st, "c_tok", [128, NB], F32)
        R.D("sync", [trim], [c_tri], out=c_tri[:], in_=trim[:])
        R.D("sync", [tokid], [c_tok], out=c_tok[:], in_=tokid[:])
        R.I("vector", "tensor_copy", [c_tri], [c_trb], out=c_trb[:], in_=c_tri[:])
        R.I("vector", "memset", [], [c_onb], c_onb[:], 1.0)
        ohs = R.sb(st, "ohs", [128, NB * NE], BF16)
        R.I("vector", "tensor_tensor", [rt_oh], [ohs], out=ohs[:].rearrange("p (b e) -> p b e", e=NE), in0=rt_oh[:, :, 0, :], in1=rt_oh[:, :, 1, :], op=ALU.add)
        rk = R.sb(st, "rk", [128, NB, NE], F32)
        cumA = R.sb(st, "cumA", [128, NB, NE], F32)
        cumB = R.sb(st, "cumB", [128, NB, NE], F32)
        tt = R.sb(st, "tt", [128, NB, NE], F32)
        pp = Rot([R.ps(st, "ppd%d" % i, [128, 512], F32) for i in range(2)])
        ncol = NB * NE
        rkf = rk[:].rearrange("p b e -> p (b e)")
        ttf = tt[:].rearrange("p b e -> p (b e)")
        for c0 in range(0, ncol, 512):
            w_ = min(512, ncol - c0)
            p_ = pp.next()
            R.I("tensor", "matmul", [c_trb, ohs], [p_], p_[:, 0:w_], c_trb[:], ohs[:, c0:c0 + w_], start=True, stop=True)
            R.I("vector", "tensor_copy", [p_], [rk], out=rkf[:, c0:c0 + w_], in_=p_[:, 0:w_])
            p_ = pp.next()
            R.I("tensor", "matmul", [c_onb, ohs], [p_], p_[:, 0:w_], c_onb[:], ohs[:, c0:c0 + w_], start=True, stop=True)
            R.I("vector", "tensor_copy", [p_], [tt], out=ttf[:, c0:c0 + w_], in_=p_[:, 0:w_])
        R.I("vector", "tensor_copy", [tt], [cumA], out=cumA[:], in_=tt[:])
        cur, oth = cumA, cumB
        sft = 1
        while sft < NB:
            R.I("vector", "tensor_copy", [cur], [oth], out=oth[:, 0:sft, :], in_=cur[:, 0:sft, :])
            R.I("vector", "tensor_tensor", [cur], [oth], out=oth[:, sft:NB, :], in0=cur[:, sft:NB, :], in1=cur[:, 0:NB - sft, :], op=ALU.add)
            cur, oth = oth, cur
            sft *= 2
        R.I("vector", "tensor_tensor", [cur, tt], [oth], out=oth[:], in0=cur[:], in1=tt[:], op=ALU.subtract)
        R.I("vector", "tensor_tensor", [oth, rk], [rk], out=rk[:], in0=oth[:], in1=rk[:], op=ALU.add)
        slf = R.sb(st, "slf", [128, NB, 2], F32)
        rkk = R.sb(st, "rkk", [128, NB], F32)
        vld = R.sb(st, "vld", [128, NB], F32)
        for k in range(2):
            R.I("vector", "tensor_tensor", [rt_oh, rk], [cur], out=cur[:], in0=rt_oh[:, :, k, :], in1=rk[:], op=ALU.mult)
            R.I("vector", "tensor_reduce", [cur], [rkk], out=rkk[:], in_=cur[:], axis=AX.X, op=ALU.add)
            R.I("vector", "tensor_scalar", [rkk], [vld], out=vld[:], in0=rkk[:], scalar1=float(CAP), scalar2=None, op0=ALU.is_lt)
            R.I("vector", "scalar_tensor_tensor", [rt_e, rkk], [rkk], out=rkk[:], in0=rt_e[:, :, k], scalar=float(CAP), in1=rkk[:], op0=ALU.mult, op1=ALU.add)
            R.I("vector", "tensor_scalar", [rkk], [rkk], out=rkk[:], in0=rkk[:], scalar1=-float(NSLOT), scalar2=None, op0=ALU.add)
            R.I("vector", "tensor_tensor", [rkk, vld], [rkk], out=rkk[:], in0=rkk[:], in1=vld[:], op=ALU.mult)
            R.I("vector", "tensor_scalar", [rkk], [slf], out=slf[:, :, k], in0=rkk[:], scalar1=float(NSLOT), scalar2=None, op0=ALU.add)
        R.I("vector", "tensor_copy", [slf], [slots_i], out=slots_i[:], in_=slf[:])
        NR = (NSLOT + 128) // 128
        ini = R.sb(st, "ini", [128, NR, 2], F32)
        R.I("vector", "memset", [], [ini], ini[:, :, 0:1], float(TOK))
        R.I("vector", "memset", [], [ini], ini[:, :, 1:2], 0.0)
        R.D("sync", [ini], [SLOTI], out=SLOTI.t.rearrange("(p r) c -> p r c", p=128), in_=ini[:])
        zf = R.sb(st, "zf", [128, D], F32)
        zb = R.sb(st, "zb", [128, D], BF16)
        R.I("vector", "memset", [], [zf], zf[:], 0.0)
        R.I("vector", "memset", [], [zb], zb[:], 0.0)
        R.D("sync", [zf], [Yd], out=Yd.t[NSLOT:NSLOT + 128, :], in_=zf[:])
        R.D("sync", [zb], [H2], out=H2.t[TOK:TOK + 128, :], in_=zb[:])
        sc = R.sb(st, "sc", [128, NB, 2, 2], F32)
        for k in range(2):
            R.I("vector", "tensor_copy", [c_tok], [sc], out=sc[:, :, k, 0], in_=c_tok[:])
            R.I("vector", "tensor_copy", [rt_w], [sc], out=sc[:, :, k, 1], in_=rt_w[:, :, k])
        for b in range(NB):
            for k in range(2):
                R.dma("gpsimd", (lambda e, b=b, k=k: e.indirect_dma_start(out=SLOTI.t[:, :], out_offset=bass.IndirectOffsetOnAxis(ap=slots_i[:, b, k:k + 1], axis=0),
                                                                          in_=sc[:, b, k, :], in_offset=None)),
                      reads=[sc, slots_i, SLOTI], writes=[SLOTI], semobj=sc)
        R.flush()

    if "noD" not in cfg.debug:
      with ExitStack() as st:
        QW = min(256, DE)
        NQ = DE // QW
        DC = DE // 128
        c_idf = R.sb(st, "c_idf", [128, 128], F32)
        c_idb = R.sb(st, "c_idb", [128, 128], BF16)
        R.D("sync", [ident], [c_idf], out=c_idf[:], in_=ident[:])
        R.I("vector", "tensor_copy", [c_idf], [c_idb], out=c_idb[:], in_=c_idf[:])
        wg = Rot([R.sb(st, "wg%d" % i, [128, 16, QW], BF16) for i in range(2)])
        wu = Rot([R.sb(st, "wu%d" % i, [128, 16, QW], BF16) for i in range(2)])
        wd = Rot([R.sb(st, "wd%d" % i, [128, DC, D], BF16) for i in range(2)])
        si = Rot([R.sb(st, "si%d" % i, [128, CAPB, 2], F32) for i in range(2)])
        idx = Rot([R.sb(st, "idx%d" % i, [128, CAPB], I32) for i in range(2)])
        xg = Rot([R.sb(st, "xg%d" % i, [128, D], BF16) for i in range(3)])
        xbT = Rot([R.sb(st, "xbT%d" % i, [128, 16, CAP], BF16) for i in range(2)])
        hidT = R.sb(st, "hidT", [128, DC, CAP], BF16)
        sil = Rot([R.sb(st, "sil%d" % i, [128, 512], F32) for i in range(2)])
        yo = Rot([R.sb(st, "yo%d" % i, [128, D], F32) for i in range(2)])
        pT = Rot([R.ps(st, "pTd%d" % i, [128, 8, 128], BF16) for i in range(2)])
        pg = Rot([R.ps(st, "pg%d" % i, [128, 512], F32) for i in range(2)])
        pu = Rot([R.ps(st, "pu%d" % i, [128, 512], F32) for i in range(2)])
        pd = Rot([R.ps(st, "pd%d" % i, [128, 512], F32) for i in range(2)])
        sgs = []
        ngrp = (CAP + 511) // 512
        gsz = CAP // ngrp
        for i in range(ngrp):
            sgs.append((i * gsz, gsz))
        evq = 0
        for e_ in range(NE):
            si_ = si.next()
            idx_ = idx.next()
            R.D("sync", [SLOTI], [si_], out=si_[:], in_=SLOTI.t[e_ * CAP:(e_ + 1) * CAP, :].rearrange("(j p) c -> p j c", p=128))
            R.I("vector", "tensor_copy", [si_], [idx_], out=idx_[:], in_=si_[:, :, 0])
            xbT_ = xbT.next()
            for j in range(CAPB):
                xg_ = xg.next()
                R.dma("gpsimd", (lambda e, j=j, xg_=xg_, idx_=idx_: e.indirect_dma_start(out=xg_[:], out_offset=None, in_=H2.t[:, :],
                                                                                      in_offset=bass.IndirectOffsetOnAxis(ap=idx_[:, j:j + 1], axis=0))),
                      reads=[H2, idx_], writes=[xg_], semobj=xg_)
                for half in range(2):
                    p = pT.next()
                    for c in range(8):
                        cc = half * 8 + c
                        R.I("tensor", "transpose", [xg_, c_idb], [p], out=p[:, c, :], in_=xg_[:, cc * 128:(cc + 1) * 128], identity=c_idb[:])
                    dst = xbT_[:, half * 8:half * 8 + 8, j * 128:(j + 1) * 128]
                    if evq % 2 == 0:
                        R.I("scalar", "activation", [p], [xbT_], out=dst, in_=p[:], func=AF.Copy)
                    else:
                        R.I("vector", "tensor_copy", [p], [xbT_], out=dst, in_=p[:])
                    evq += 1
            wgv = w_gate.t[e_].rearrange("(c p) n -> p c n", p=128)
            wuv = w_up.t[e_].rearrange("(c p) n -> p c n", p=128)
            wdv = w_down.t[e_].rearrange("(c p) n -> p c n", p=128)
            wd_ = wd.next()
            for c0 in range(0, DC, 2):
                R.D("gpsimd", [w_down], [wd_], out=wd_[:, c0:c0 + 2, :], in_=wdv[:, c0:c0 + 2, :])
            for qq in range(NQ):
                wg_, wu_ = wg.next(), wu.next()
                for c0 in range(0, 16, 8):
                    R.D("gpsimd", [w_gate], [wg_], out=wg_[:, c0:c0 + 8, :], in_=wgv[:, c0:c0 + 8, qq * QW:(qq + 1) * QW])
                    R.D("gpsimd", [w_up], [wu_], out=wu_[:, c0:c0 + 8, :], in_=wuv[:, c0:c0 + 8, qq * QW:(qq + 1) * QW])
                for ct in range(QW // 128):
                    hc = qq * (QW // 128) + ct
                    for (s0, sn) in sgs:
                        pg_, pu_ = pg.next(), pu.next()
                        for c in range(16):
                            R.I("tensor", "matmul", [wg_, xbT_], [pg_], pg_[:, 0:sn], wg_[:, c, ct * 128:(ct + 1) * 128], xbT_[:, c, s0:s0 + sn], start=(c == 0), stop=(c == 15))
                        for c in range(16):
                            R.I("tensor", "matmul", [wu_, xbT_], [pu_], pu_[:, 0:sn], wu_[:, c, ct * 128:(ct + 1) * 128], xbT_[:, c, s0:s0 + sn], start=(c == 0), stop=(c == 15))
                        sl_ = sil.next()
                        R.I("scalar", "activation", [pg_], [sl_], out=sl_[:, 0:sn], in_=pg_[:, 0:sn], func=AF.Silu)
                        R.I("vector", "tensor_tensor", [sl_, pu_], [hidT], out=hidT[:, hc, s0:s0 + sn], in0=sl_[:, 0:sn], in1=pu_[:, 0:sn], op=ALU.mult)
            for sb_ in range(CAPB):
                yo_ = yo.next()
                for colt in range(4):
                    pd_ = pd.next()
                    for cc in range(DC):
                        R.I("tensor", "matmul", [hidT, wd_], [pd_], pd_[:], hidT[:, cc, sb_ * 128:(sb_ + 1) * 128], wd_[:, cc, colt * 512:(colt + 1) * 512],
                            start=(cc == 0), stop=(cc == DC - 1))
                    if colt % 2 == 0:
                        R.I("vector", "tensor_scalar", [pd_, si_], [yo_], out=yo_[:, colt * 512:(colt + 1) * 512], in0=pd_[:], scalar1=si_[:, sb_, 1:2], scalar2=None, op0=ALU.mult)
                    else:
                        R.I("scalar", "activation", [pd_, si_], [yo_], out=yo_[:, colt * 512:(colt + 1) * 512], in_=pd_[:], func=AF.Copy, scale=si_[:, sb_, 1:2])
                r0 = e_ * CAP + sb_ * 128
                R.D("sync", [yo_], [Yd], out=Yd.t[r0:r0 + 128, :], in_=yo_[:])
        R.flush()

    if "noE" not in cfg.debug:
      with ExitStack() as st:
        xa = Rot([R.sb(st, "xa%d" % i, [128, D], F32) for i in range(2)])
        g0 = Rot([R.sb(st, "g0_%d" % i, [128, D], F32) for i in range(2)])
        g1 = Rot([R.sb(st, "g1_%d" % i, [128, D], F32) for i in range(2)])
        for b in range(NB):
            xa_, g0_, g1_ = xa.next(), g0.next(), g1.next()
            R.D("sync", [X1], [xa_], out=xa_[:], in_=X1.t[b * 128:(b + 1) * 128, :])
            for k, g_ in ((0, g0_), (1, g1_)):
                R.dma("gpsimd", (lambda e, b=b, k=k, g_=g_: e.indirect_dma_start(out=g_[:], out_offset=None, in_=Yd.t[:, :],
                                                                              in_offset=bass.IndirectOffsetOnAxis(ap=slots_i[:, b, k:k + 1], axis=0))),
                      reads=[Yd, slots_i], writes=[g_], semobj=g_)
            R.I("vector", "tensor_tensor", [xa_, g0_], [xa_], out=xa_[:], in0=xa_[:], in1=g0_[:], op=ALU.add)
            R.I("vector", "tensor_tensor", [xa_, g1_], [xa_], out=xa_[:], in0=xa_[:], in1=g1_[:], op=ALU.add)
            R.D("sync", [xa_], [y_out], out=y_out.t[b * 128:(b + 1) * 128, :], in_=xa_[:])
        R.flush()

    if "H1" in cfg.debug:
        with ExitStack() as st:
            dtp = R.dram("dbg_taps", [3, 2, 1024, N], BF16, kind="ExternalOutput")
            drn = R.dram("dbg_rn", [128, 16], F32, kind="ExternalOutput")
            duc = R.dram("dbg_uc", [3072, 2, TOK], BF16, kind="ExternalOutput")
            tb_ = R.sb(st, "tbd", [128, N], BF16)
            for f in range(3):
                for o in range(2):
                    for ct in range(8):
                        R.D("sync", [TAPS], [tb_], out=tb_[:], in_=TAPS.t[f, o, ct * 128:(ct + 1) * 128, :])
                        R.D("sync", [tb_], [dtp], out=dtp.t[f, o, ct * 128:(ct + 1) * 128, :], in_=tb_[:])
            for j in range(24):
                for ch in range(2):
                    R.D("sync", [UC], [tb_], out=tb_[:, 0:TOK], in_=UC.t[j * 128:(j + 1) * 128, ch, :])
                    R.D("sync", [tb_], [duc], out=duc.t[j * 128:(j + 1) * 128, ch, :], in_=tb_[:, 0:TOK])
            tr_ = R.sb(st, "trd", [128, 16], F32)
            R.D("sync", [RN], [tr_], out=tr_[:], in_=RN.t)
            R.D("sync", [tr_], [drn], out=drn.t, in_=tr_[:])
            R.flush()
    if "B" in cfg.debug:
        with ExitStack() as st:
            dx1 = R.dram("dbg_x1", [TOK, D], F32, kind="ExternalOutput")
            drw = R.dram("dbg_rw", [128, NB, 2], F32, kind="ExternalOutput")
            dre = R.dram("dbg_re", [128, NB, 2], F32, kind="ExternalOutput")
            tb = R.sb(st, "tb", [128, D], F32)
            for b in range(NB):
                R.D("sync", [X1], [tb], out=tb[:], in_=X1.t[b * 128:(b + 1) * 128, :])
                R.D("sync", [tb], [dx1], out=dx1.t[b * 128:(b + 1) * 128, :], in_=tb[:])
            R.D("sync", [rt_w], [drw], out=drw.t, in_=rt_w[:])
            R.D("sync", [rt_e], [dre], out=dre.t, in_=rt_e[:])
            R.flush()
    ost.close()
    es.close()
    return nc


def rope_tables(pos):
    half = 8
    inv = np.power(np.float32(500000.0), -np.arange(half, dtype=np.float32) * np.float32(2.0) / np.float32(16.0)).astype(np.float32)
    ang = pos.astype(np.float32)[None, :] * inv[:, None]
    c = np.cos(ang).astype(np.float32)
    s = np.sin(ang).astype(np.float32)
    n = pos.shape[0]
    C = np.ones((64, n), np.float32)
    S = np.zeros((64, n), np.float32)
    C[0:8] = c
    C[8:16] = c
    S[0:8] = -s
    S[8:16] = s
    return np.concatenate([C, C], 0), np.concatenate([S, S], 0)


def const_tables():
    ident = np.eye(128, dtype=np.float32)
    rot = np.zeros((128, 128), np.float32)
    for m in range(128):
        d = m % 64
        if d < 8:
            rot[m + 8, m] = 1.0
        elif d < 16:
            rot[m - 8, m] = 1.0
    blk = np.zeros((128, 128), np.float32)
    blk[0:64, 0:64] = 1.0
    blk[64:128, 64:128] = 1.0
    return ident, rot, blk


def fft_tables(TOK):
    N = 2 * TOK
    NHI = N // 128
    KL = NHI // 2 + 1
    NB = TOK // 128
    f64 = np.float64
    nh = np.arange(NHI, dtype=f64)[:, None]
    kl = np.arange(KL, dtype=f64)[None, :]
    th = 2 * np.pi * nh * kl / NHI
    m_f1 = np.concatenate([np.cos(th), -np.sin(th)], 1).astype(np.float32)
    nl = np.arange(128, dtype=f64)[:, None, None]
    klo = np.arange(KL, dtype=f64)[None, :, None]
    kh = np.arange(128, dtype=f64)[None, None, :]
    th = 2 * np.pi * (nl * klo / N + nl * kh / 128.0)
    m_f2 = np.stack([np.cos(th), -np.sin(th)], 2).astype(np.float32)
    khh = np.arange(128, dtype=f64)[:, None]
    nll = np.arange(128, dtype=f64)[None, :]
    th = 2 * np.pi * khh * nll / 128.0
    cr, ci = np.cos(th), np.sin(th)
    m_i1 = np.stack([np.concatenate([cr, ci], 1), np.concatenate([-ci, cr], 1)], 1).astype(np.float32)
    wgt = np.full((KL,), 2.0)
    wgt[0] = 1.0
    wgt[KL - 1] = 1.0
    klo = np.arange(KL, dtype=f64)[:, None, None]
    nl = np.arange(128, dtype=f64)[None, :, None]
    nhi = np.arange(NB, dtype=f64)[None, None, :]
    th = 2 * np.pi * (nl * klo / N + nhi * klo / NHI)
    sc = (wgt / N)[:, None, None]
    m_i2 = np.stack([sc * np.cos(th), -sc * np.sin(th)], 2).astype(np.float32)
    return (np.ascontiguousarray(m_f1), np.ascontiguousarray(m_f2), np.ascontiguousarray(m_i1), np.ascontiguousarray(m_i2))


def filter_tables(TOK, L, h):
    f32 = np.float32
    N = 2 * TOK
    n = np.arange(N)
    t_lin = np.linspace(0.0, 1.0, L, dtype=f32)
    fr = np.linspace(1e-4, 15.0, 16, dtype=f32)

    def feats(lags):
        t = t_lin[lags]
        w = (f32(2.0 * math.pi) * lags.astype(f32) / f32(L)).astype(f32)
        ang = (w[:, None] * fr[None, :]).astype(f32)
        return np.concatenate([t[:, None], np.cos(ang), -np.sin(ang)], 1).astype(f32).T

    zt = np.zeros((3, 33, N), f32)
    tt = np.full((3, N), 1.0e4, f32)
    cnt = np.zeros((3, N), f32)
    dsel = np.zeros((3, 2, 2), f32)
    dsel[:, :, 0] = 1.0
    lag = np.where(n < TOK, n, N - n)
    lag[TOK] = 0
    valid = n != TOK
    zt[0] = feats(lag)
    tt[0][valid] = t_lin[lag[valid]]
    cnt[0][valid] = 1.0
    dsel[0, 0] = (1.0, 0.0)
    dsel[0, 1] = (0.0, 1.0)
    if L == 2 * TOK:
        lag_f = np.where(n < TOK, TOK + n, n - TOK)
        lag_f[TOK] = 0
        cnt_f = ((n < TOK)).astype(f32)
        lag_b = np.where(n < TOK, TOK - n, N + TOK - n)
        lag_b[TOK] = 0
        cnt_b = ((n == 0) | (n > TOK)).astype(f32)
        kinds = ("b", "f") if h == 0 else ("f", "b")
        for fi, kd in zip((1, 2), kinds):
            lg = lag_f if kd == "f" else lag_b
            zt[fi] = feats(lg)
            tt[fi][valid] = t_lin[lg[valid]]
            cnt[fi] = cnt_f if kd == "f" else cnt_b
            cnt[fi][TOK] = 0.0
            dsel[fi, :, :] = (1.0, 0.0) if kd == "f" else (0.0, 1.0)
    NPT = N // 512
    tfl = np.zeros((3 * NPT + 3,), f32)
    tfl[0:NPT] = 1.0
    if L == 2 * TOK:
        kinds = ("b", "f") if h == 0 else ("f", "b")
        for fi, kd in zip((1, 2), kinds):
            for pt in range(NPT):
                if kd == "f":
                    tfl[fi * NPT + pt] = 1.0 if pt * 512 < TOK else 0.0
                else:
                    tfl[fi * NPT + pt] = 1.0 if pt * 512 >= TOK else 0.0
            if kd == "b":
                tfl[3 * NPT + fi] = 1.0
    cnt = np.ascontiguousarray(np.broadcast_to(tfl[None, :], (128, 3 * NPT + 3)))
    return zt, tt, cnt, dsel


def prep(cfg, inp):
    TOK, TOKX, D = cfg.TOK, cfg.TOKX, cfg.D
    ident, rot, blk = const_tables()
    f32 = np.float32
    xp = np.asarray(inp["x_prompt"], f32)
    xs = np.asarray(inp["x_sample"], f32)
    w_in = np.ascontiguousarray(np.asarray(inp["w_in"], f32)[0])
    n1w = np.ascontiguousarray(np.broadcast_to(np.asarray(inp["norm1_w"], f32)[0][None, :], (128, D)))
    qkw = np.stack([np.tile(np.asarray(inp["q_norm_w"], f32)[0], 2), np.tile(np.asarray(inp["k_norm_w"], f32)[0], 2)], 1)
    NG, NE, EPG = cfg.NG, cfg.NE, cfg.EPG
    w_out = np.ascontiguousarray(np.asarray(inp["w_out"], f32)[0])
    aon = np.ascontiguousarray(np.asarray(inp["attn_out_norm_w"], f32)[0].reshape(8, 128).T)
    hyn = np.ascontiguousarray(np.asarray(inp["hy_out_norm_w"], f32)[0].reshape(8, 128).T)
    n2w = np.ascontiguousarray(np.broadcast_to(np.asarray(inp["norm2_w"], f32)[0][None, :], (128, D)))
    w_r = np.ascontiguousarray(np.concatenate([np.asarray(inp["w_route_group"], f32)[0], np.asarray(inp["w_route_expert"], f32)[0]], 1))
    b_r = np.concatenate([np.asarray(inp["b_route_group"], f32)[0], np.asarray(inp["b_route_expert"], f32)[0]])
    b_r = np.ascontiguousarray(np.broadcast_to(b_r[None, :], (128, NG + NE)))
    iota_e = np.ascontiguousarray(np.broadcast_to(np.arange(NE, dtype=f32)[None, :], (128, NE)))
    sink = np.asarray(inp["attn_sink"], f32)[0]
    sink2 = np.zeros((128, 8), f32)
    for g in range(4):
        for j in range(2):
            sink2[:64, g * 2 + j] = sink[4 * g + 2 * j]
            sink2[64:, g * 2 + j] = sink[4 * g + 2 * j + 1]
    m_f1, m_f2, m_i1, m_i2 = fft_tables(TOK)
    cw = np.asarray(inp["conv_w"], f32)[0]
    cb = np.asarray(inp["conv_b"], f32)[0]
    cwt = np.ascontiguousarray(np.stack([cw[0], cw[1], cw[2], cb], 1).reshape(24, 128, 4).transpose(1, 0, 2))
    f_vec = np.ascontiguousarray(np.stack([np.asarray(inp[k], f32)[0] for k in ("filt_freq", "filt_b1", "filt_b2", "filt_b3")], 1))
    max_decay = math.log(1e-2) / 0.3
    min_decay = math.log(1e-2) / 1.5
    deltas = np.abs(np.linspace(min_decay, max_decay, 1024, dtype=f32))
    f_delta = np.ascontiguousarray(deltas.reshape(8, 128).T)
    hyb = np.ascontiguousarray(np.asarray(inp["hy_bias"], f32)[0].reshape(2, 16, 64).transpose(2, 0, 1))
    trim = (np.arange(128)[:, None] < np.arange(128)[None, :]).astype(f32)
    tokid = (np.arange(128)[:, None] + 128 * np.arange(cfg.NB)[None, :]).astype(f32)
    w_gate = np.ascontiguousarray(np.asarray(inp["w_gate"], f32)[0])
    w_up = np.ascontiguousarray(np.asarray(inp["w_up"], f32)[0])
    w_down = np.ascontiguousarray(np.asarray(inp["w_down"], f32)[0])
    jj = np.arange(128)[:, None]
    qi = np.arange(128)[None, :]
    tri_prev = np.tile((jj >= qi).astype(f32), (1, 4))
    tri_next = np.tile((jj <= qi).astype(f32), (1, 4))
    maps = []
    for c in range(8):
        if c < 4:
            seq, h, L = xp[c], 0, TOK
        else:
            seq, h, L = xs[(c - 4) // 2], (c - 4) % 2, 2 * TOK
        base = h * TOK
        x_own = np.zeros((TOKX, D), f32)
        x_own[:TOK] = seq[base:base + TOK]
        if base >= 128:
            x_own[TOK:TOK + 128] = seq[base - 128:base]
        if base + TOK + 128 <= L:
            x_own[TOK + 128:TOK + 256] = seq[base + TOK:base + TOK + 128]
        x_oth = np.zeros((TOK, D), f32)
        if L > TOK:
            x_oth[:] = seq[(1 - h) * TOK:(2 - h) * TOK]
        pos = np.concatenate([base + np.arange(TOK), base - 128 + np.arange(128), base + TOK + np.arange(128), np.zeros(256)]).astype(f32)
        ct, stb = rope_tables(pos)
        has_prev = base >= 128
        has_next = base + TOK + 128 <= L
        mk = np.stack([tri_prev, tri_next, tri_prev * (1.0 if has_prev else 0.0), tri_next * (1.0 if has_next else 0.0)]).astype(f32)
        zt_, tt_, cnt_, dsel_ = filter_tables(TOK, L, h)
        two = (L == 2 * TOK)
        ef = np.array([1.0 if h == 1 else 0.0, 1.0 if (two and h == 0) else 0.0, 1.0 if (two and h == 0) else 0.0, 1.0 if h == 1 else 0.0], f32)
        m = dict(cwt=cwt, eflag=np.ascontiguousarray(np.broadcast_to(ef[None, :], (128, 4))), f_w1=np.ascontiguousarray(np.asarray(inp["filt_w1"], f32)[0]),
                 f_w2=np.ascontiguousarray(np.asarray(inp["filt_w2"], f32)[0]), f_w3=np.ascontiguousarray(np.asarray(inp["filt_w3"], f32)[0]),
                 f_w4=np.ascontiguousarray(np.asarray(inp["filt_w4"], f32)[0]), f_vec=f_vec, f_zt=zt_, f_tt=tt_, f_cnt=cnt_,
                 f_dsel=np.ascontiguousarray(np.broadcast_to(dsel_.reshape(1, 12), (64, 12))), f_delta=f_delta, hyb=hyb,
                 m_f1=m_f1, m_f2=m_f2, m_i1=m_i1, m_i2=m_i2, trim=trim, tokid=tokid, w_gate=w_gate, w_up=w_up, w_down=w_down, masks=mk, sink2=sink2, w_out=w_out, aonw=aon, hynw=hyn, n2w=n2w, w_r=w_r, b_r=b_r, iota_e=iota_e, x_own=x_own, x_oth=x_oth, w_in=w_in, n1w=n1w, ident=ident, rotm=rot, blk1=blk, qkw=np.ascontiguousarray(qkw),
                 cos_t=np.ascontiguousarray(ct), sin_t=np.ascontiguousarray(stb))
        maps.append(m)
    return maps


_CACHE = {}


def kernel(**inputs):
    cfg = Cfg()
    if "nc" not in _CACHE:
        _CACHE["nc"] = build(cfg)
    nc = _CACHE["nc"]
    maps = prep(cfg, inputs)
    res = run_bass_kernel_spmd(nc, maps, core_ids=list(range(8)))
    outs = [np.asarray(r["y"], np.float32) for r in res.results]
    TOK = cfg.TOK
    y_prompt = np.stack(outs[0:4], 0)
    y_sample = np.stack([np.concatenate([outs[4], outs[5]], 0), np.concatenate([outs[6], outs[7]], 0)], 0)
    return (y_prompt, y_sample)
```
